# Optimizing a Trainium2 kernel written in Bass

```python
import math
import jax, jax.numpy as jnp
from jax import lax
import numpy as np

D_MODEL = 2048
BATCH = 4
SEQ = 2048
DEPTH = 4

DA_HEADS = 4
DA_QK_DIM = 64
DA_V_DIM = 128
Q_BLOCK = 128
HG_HEADS = 6
HG_K_DIM = 128
HG_V_DIM = 128
HG_CHUNK = 16
NSA_HEADS = 6
NSA_KV_HEADS = 2
NSA_GROUP = NSA_HEADS // NSA_KV_HEADS
NSA_DIM = 128
CMP_LEN = 32
CMP_STRIDE = 16
CMP_HIDDEN = 256
SLC_BLOCK = 64
SLC_TOPK = 16
SLC_Q_BLOCK = 64
WINDOW = 512
WIN_Q_BLOCK = 128
FORCE_BONUS = 1.0e4
D_A = DA_HEADS * DA_V_DIM
D_B = HG_HEADS * HG_V_DIM
D_C = NSA_HEADS * NSA_DIM
D_MIX = D_A + D_B + D_C
D_FF = ((8 * D_MODEL // 3 + 255) // 256) * 256
IN_SIZES = (
    DA_HEADS * 2 * DA_QK_DIM, DA_HEADS * 2 * DA_QK_DIM, D_A,
    HG_HEADS * HG_K_DIM, HG_HEADS * HG_K_DIM, D_B, D_B,
    D_C,
    NSA_KV_HEADS * NSA_DIM, NSA_KV_HEADS * NSA_DIM,
    NSA_KV_HEADS * NSA_DIM, NSA_KV_HEADS * NSA_DIM,
    NSA_KV_HEADS * NSA_DIM, NSA_KV_HEADS * NSA_DIM,
    NSA_HEADS * 3,
)
D_IN = sum(IN_SIZES)

kernel_name = "hymba_diffattn_hgrn2_nsa_trunk"


def rmsnorm(x, g, eps=1e-6):
    xf = x.astype(jnp.float32)
    y = xf * lax.rsqrt(jnp.mean(xf * xf, axis=-1, keepdims=True) + eps)
    return (y * g.astype(jnp.float32)).astype(x.dtype)


def masked_softmax(s, mask):
    s = jnp.where(mask, s.astype(jnp.float32), -jnp.inf)
    m = jnp.max(s, axis=-1, keepdims=True)
    m = jnp.where(jnp.isfinite(m), m, 0.0)
    p = jnp.exp(s - m)
    return p / jnp.maximum(jnp.sum(p, axis=-1, keepdims=True), 1e-30)


def diff_attention(q, k, v, lam):
    B, T = q.shape[:2]
    nblk = T // Q_BLOCK
    scale = DA_QK_DIM ** -0.5
    qb = jnp.moveaxis(q.reshape(B, nblk, Q_BLOCK, DA_HEADS, 2, DA_QK_DIM), 1, 0)
    kpos = jnp.arange(T)

    def block(args):
        qi, i = args
        s = jnp.einsum('bqhcd,bkhcd->bhcqk', qi, k) * scale
        qpos = i * Q_BLOCK + jnp.arange(Q_BLOCK)
        p = masked_softmax(s, kpos[None, :] <= qpos[:, None])
        pd = p[:, :, 0] - lam * p[:, :, 1]
        return jnp.einsum('bhqk,bkhd->bqhd', pd.astype(v.dtype), v)

    o = lax.map(block, (qb, jnp.arange(nblk)))
    return jnp.moveaxis(o, 0, 1).reshape(B, T, DA_HEADS, DA_V_DIM)


def hgrn2_mixer(f_logit, q, i, lb):
    B, T = f_logit.shape[:2]
    n = T // HG_CHUNK
    lb = lb.astype(jnp.float32).reshape(HG_HEADS, HG_K_DIM)
    logf = jnp.logaddexp(jnp.log(lb), jnp.log1p(-lb) + jax.nn.log_sigmoid(f_logit.astype(jnp.float32)))
    key = -jnp.expm1(logf)

    def to_chunks(a):
        return a.astype(jnp.float32).reshape(B, n, HG_CHUNK, HG_HEADS, -1).transpose(0, 3, 1, 2, 4)

    logf_c, q_c, k_c, v_c = to_chunks(logf), to_chunks(q), to_chunks(key), to_chunks(i)
    cum = jnp.cumsum(logf_c, axis=3)
    causal = jnp.tril(jnp.ones((HG_CHUNK, HG_CHUNK), bool))[:, :, None]
    diff = cum[:, :, :, :, None, :] - cum[:, :, :, None, :, :]
    decay = jnp.where(causal, jnp.exp(jnp.where(causal, diff, 0.0)), 0.0)
    scores = jnp.einsum('bhntk,bhnsk,bhntsk->bhnts', q_c, k_c, decay)
    o_intra = jnp.einsum('bhnts,bhnsv->bhntv', scores, v_c)
    last = cum[:, :, :, -1:, :]
    d_state = jnp.einsum('bhnsk,bhnsv->bhnkv', k_c * jnp.exp(last - cum), v_c)
    chunk_decay = jnp.exp(last[:, :, :, 0, :])

    def step(S, inp):
        dec, dS = inp
        return dec[..., None] * S + dS, S

    S0 = jnp.zeros((B, HG_HEADS, HG_K_DIM, HG_V_DIM), jnp.float32)
    _, S_prev = lax.scan(step, S0, (jnp.moveaxis(chunk_decay, 2, 0), jnp.moveaxis(d_state, 2, 0)))
    S_prev = jnp.moveaxis(S_prev, 0, 2)
    o_inter = jnp.einsum('bhntk,bhnkv->bhntv', q_c * jnp.exp(cum), S_prev)
    return (o_intra + o_inter).transpose(0, 2, 3, 1, 4).reshape(B, T, HG_HEADS, HG_V_DIM)


def compress_blocks(kv, cidx, pe, w1, w2):
    B = kv.shape[0]
    n_cmp = cidx.shape[0]
    blocks = kv[:, cidx] + pe[None, None, :, None, :]
    flat = blocks.transpose(0, 3, 1, 2, 4).reshape(B, NSA_KV_HEADS, n_cmp, CMP_LEN * NSA_DIM)
    return jax.nn.gelu(flat @ w1) @ w2


def nsa_mixer(q, k_cmp, v_cmp, k_slc, v_slc, k_win, v_win, gate_logit, pe_k, pe_v, ck_w1, ck_w2, cv_w1, cv_w2):
    B, T = q.shape[:2]
    G, J, D = NSA_KV_HEADS, NSA_GROUP, NSA_DIM
    scale = D ** -0.5
    q = q.reshape(B, T, G, J, D)
    split_kv = lambda a: a.reshape(B, T, G, D)
    k_cmp, v_cmp, k_slc, v_slc, k_win, v_win = map(split_kv, (k_cmp, v_cmp, k_slc, v_slc, k_win, v_win))
    tpos = jnp.arange(T)

    n_cmp = (T - CMP_LEN) // CMP_STRIDE + 1
    cidx = np.arange(n_cmp)[:, None] * CMP_STRIDE + np.arange(CMP_LEN)[None, :]
    kc = compress_blocks(k_cmp, cidx, pe_k, ck_w1, ck_w2)
    vc = compress_blocks(v_cmp, cidx, pe_v, cv_w1, cv_w2)
    s_c = jnp.einsum('btgjd,bgnd->bgjtn', q, kc) * scale
    cmp_mask = jnp.asarray(cidx[:, -1])[None, :] <= tpos[:, None]
    p_c = masked_softmax(s_c, cmp_mask)
    o_cmp = jnp.einsum('bgjtn,bgnd->btgjd', p_c.astype(vc.dtype), vc)

    n_sel = T // SLC_BLOCK
    c_start = np.arange(n_cmp) * CMP_STRIDE
    s_start = np.arange(n_sel) * SLC_BLOCK
    overlap = ((c_start[:, None] < s_start[None, :] + SLC_BLOCK)
               & (c_start[:, None] + CMP_LEN > s_start[None, :])).astype(np.float32)
    imp = jnp.einsum('bgjtn,ns->bgts', p_c, jnp.asarray(overlap))
    blk = jnp.arange(n_sel)[None, :]
    cur = (tpos // SLC_BLOCK)[:, None]
    forced = (blk == 0) | (blk == cur) | (blk == cur - 1)
    valid = blk * SLC_BLOCK <= tpos[:, None]
    score = jnp.where(valid, imp + jnp.where(forced, FORCE_BONUS, 0.0), -jnp.inf)
    top = min(SLC_TOPK, n_sel)
    _, sel_idx = lax.top_k(score, top)

    kblk = k_slc.reshape(B, n_sel, SLC_BLOCK, G, D).transpose(0, 3, 1, 2, 4)
    vblk = v_slc.reshape(B, n_sel, SLC_BLOCK, G, D).transpose(0, 3, 1, 2, 4)
    nq = T // SLC_Q_BLOCK
    qc = jnp.moveaxis(q.reshape(B, nq, SLC_Q_BLOCK, G, J, D), 1, 0)
    idxc = jnp.moveaxis(sel_idx.reshape(B, G, nq, SLC_Q_BLOCK, top), 2, 0)
    bi = jnp.arange(B)[:, None, None, None]
    gi = jnp.arange(G)[None, :, None, None]

    def sel_block(args):
        qi, ii, c = args
        ksel = kblk[bi, gi, ii]
        vsel = vblk[bi, gi, ii]
        s = jnp.einsum('bqgjd,bgqnld->bgjqnl', qi, ksel) * scale
        s = s.reshape(B, G, J, SLC_Q_BLOCK, top * SLC_BLOCK)
        kpos = ii[..., None] * SLC_BLOCK + jnp.arange(SLC_BLOCK)
        qpos = c * SLC_Q_BLOCK + jnp.arange(SLC_Q_BLOCK)
        mask = (kpos <= qpos[None, None, :, None, None]).reshape(B, G, 1, SLC_Q_BLOCK, top * SLC_BLOCK)
        p = masked_softmax(s, mask)
        return jnp.einsum('bgjqm,bgqmd->bqgjd', p.astype(vsel.dtype),
                          vsel.reshape(B, G, SLC_Q_BLOCK, top * SLC_BLOCK, D))

    o_slc = lax.map(sel_block, (qc, idxc, jnp.arange(nq)))
    o_slc = jnp.moveaxis(o_slc, 0, 1).reshape(B, T, G, J, D)

    nw = T // WIN_Q_BLOCK
    span = WINDOW + WIN_Q_BLOCK
    widx = np.arange(nw)[:, None] * WIN_Q_BLOCK + np.arange(span)[None, :]
    pad = ((0, 0), (WINDOW, 0), (0, 0), (0, 0))
    kwb = jnp.pad(k_win, pad)[:, widx]
    vwb = jnp.pad(v_win, pad)[:, widx]
    qw = q.reshape(B, nw, WIN_Q_BLOCK, G, J, D)
    s_w = jnp.einsum('bnqgjd,bnkgd->bgjnqk', qw, kwb) * scale
    kpos_w = widx - WINDOW
    qpos_w = np.arange(nw)[:, None] * WIN_Q_BLOCK + np.arange(WIN_Q_BLOCK)[None, :]
    dist = qpos_w[:, :, None] - kpos_w[:, None, :]
    p_w = masked_softmax(s_w, jnp.asarray((dist >= 0) & (dist < WINDOW)))
    o_win = jnp.einsum('bgjnqk,bnkgd->bnqgjd', p_w.astype(vwb.dtype), vwb).reshape(B, T, G, J, D)

    g = jax.nn.sigmoid(gate_logit.reshape(B, T, G, J, 3))
    o = g[..., 0:1] * o_cmp + g[..., 1:2] * o_slc + g[..., 2:3] * o_win
    return o.reshape(B, T, D_C)


def setup_inputs(seed: int = 0) -> dict:
    key = jax.random.key(seed)
    ks = jax.random.split(key, 24)
    f32 = jnp.float32

    def nrm(k, shape, scale):
        return jax.random.normal(k, shape, f32) * scale

    def gain(k, shape):
        return 1.0 + 0.01 * jax.random.normal(k, shape, f32)

    cmp_in = CMP_LEN * NSA_DIM
    return {
        "x": nrm(ks[0], (BATCH, SEQ, D_MODEL), 1.0),
        "attn_norm": gain(ks[1], (DEPTH, D_MODEL)),
        "w_in": nrm(ks[2], (DEPTH, D_MODEL, D_IN), D_MODEL ** -0.5),
        "da_lam_q1": nrm(ks[3], (DEPTH, DA_QK_DIM), 0.1),
        "da_lam_k1": nrm(ks[4], (DEPTH, DA_QK_DIM), 0.1),
        "da_lam_q2": nrm(ks[5], (DEPTH, DA_QK_DIM), 0.1),
        "da_lam_k2": nrm(ks[6], (DEPTH, DA_QK_DIM), 0.1),
        "da_norm": gain(ks[7], (DEPTH, DA_HEADS, DA_V_DIM)),
        "hg_gamma": nrm(ks[8], (DEPTH, HG_HEADS * HG_K_DIM), 0.5),
        "hg_norm": gain(ks[9], (DEPTH, HG_HEADS, HG_V_DIM)),
        "nsa_pe_k": nrm(ks[10], (DEPTH, CMP_LEN, NSA_DIM), 0.1),
        "nsa_pe_v": nrm(ks[11], (DEPTH, CMP_LEN, NSA_DIM), 0.1),
        "nsa_ck_w1": nrm(ks[12], (DEPTH, cmp_in, CMP_HIDDEN), cmp_in ** -0.5),
        "nsa_ck_w2": nrm(ks[13], (DEPTH, CMP_HIDDEN, NSA_DIM), CMP_HIDDEN ** -0.5),
        "nsa_cv_w1": nrm(ks[14], (DEPTH, cmp_in, CMP_HIDDEN), cmp_in ** -0.5),
        "nsa_cv_w2": nrm(ks[15], (DEPTH, CMP_HIDDEN, NSA_DIM), CMP_HIDDEN ** -0.5),
        "w_out": nrm(ks[16], (DEPTH, D_MIX, D_MODEL), D_MIX ** -0.5),
        "ffn_norm": gain(ks[17], (DEPTH, D_MODEL)),
        "w_gate": nrm(ks[18], (DEPTH, D_MODEL, D_FF), D_MODEL ** -0.5),
        "w_up": nrm(ks[19], (DEPTH, D_MODEL, D_FF), D_MODEL ** -0.5),
        "w_down": nrm(ks[20], (DEPTH, D_FF, D_MODEL), D_FF ** -0.5),
        "final_norm": gain(ks[21], (D_MODEL,)),
    }


def reference(x, attn_norm, w_in, da_lam_q1, da_lam_k1, da_lam_q2, da_lam_k2, da_norm, hg_gamma, hg_norm,
              nsa_pe_k, nsa_pe_v, nsa_ck_w1, nsa_ck_w2, nsa_cv_w1, nsa_cv_w2, w_out, ffn_norm,
              w_gate, w_up, w_down, final_norm):
    B, T, _ = x.shape
    offsets = np.cumsum(IN_SIZES)[:-1].tolist()
    lbs = jnp.cumsum(jax.nn.softmax(hg_gamma.astype(jnp.float32), axis=0), axis=0)
    lbs = lbs - lbs[0:1]
    for l in range(DEPTH):
        h = rmsnorm(x, attn_norm[l])
        (a_q, a_k, a_v, b_f, b_q, b_i, b_g, c_q, c_kc, c_vc, c_ks, c_vs, c_kw, c_vw, c_g) = \
            jnp.split(h @ w_in[l], offsets, axis=-1)

        lam_init = 0.8 - 0.6 * math.exp(-0.3 * l)
        lam = (jnp.exp(jnp.sum(da_lam_q1[l].astype(jnp.float32) * da_lam_k1[l].astype(jnp.float32)))
               - jnp.exp(jnp.sum(da_lam_q2[l].astype(jnp.float32) * da_lam_k2[l].astype(jnp.float32)))
               + lam_init)
        oa = diff_attention(a_q.reshape(B, T, DA_HEADS, 2, DA_QK_DIM),
                            a_k.reshape(B, T, DA_HEADS, 2, DA_QK_DIM),
                            a_v.reshape(B, T, DA_HEADS, DA_V_DIM), lam)
        ya = (rmsnorm(oa, da_norm[l]) * (1.0 - lam_init)).reshape(B, T, D_A).astype(x.dtype)

        ob = hgrn2_mixer(b_f.reshape(B, T, HG_HEADS, HG_K_DIM), b_q.reshape(B, T, HG_HEADS, HG_K_DIM),
                         b_i.reshape(B, T, HG_HEADS, HG_V_DIM), lbs[l])
        yb = (rmsnorm(ob, hg_norm[l]) * jax.nn.silu(b_g.reshape(B, T, HG_HEADS, HG_V_DIM).astype(jnp.float32)))
        yb = yb.reshape(B, T, D_B).astype(x.dtype)

        yc = nsa_mixer(c_q, c_kc, c_vc, c_ks, c_vs, c_kw, c_vw, c_g, nsa_pe_k[l], nsa_pe_v[l],
                       nsa_ck_w1[l], nsa_ck_w2[l], nsa_cv_w1[l], nsa_cv_w2[l]).astype(x.dtype)

        x = x + jnp.concatenate([ya, yb, yc], axis=-1) @ w_out[l]
        h = rmsnorm(x, ffn_norm[l])
        x = x + (jax.nn.silu(h @ w_gate[l]) * (h @ w_up[l])) @ w_down[l]
    return rmsnorm(x, final_norm)
```

```python
import contextlib
import math
import numpy as np
import ml_dtypes
import concourse.bass as bass
import concourse.mybir as mybir
from concourse.bass_utils import run_bass_kernel_spmd

F32 = mybir.dt.float32
BF16 = mybir.dt.bfloat16
AF = mybir.ActivationFunctionType
ALU = mybir.AluOpType
AX = mybir.AxisListType

T = 2048
D = 2048
DIN = 6930
DFF = 5632
DEPTH = 4
NT = T // 128
EPS = 1e-6
NEG = -30000.0

SEG = [
    ("a_q", 0, 512, "FM", "b"), ("a_k", 512, 512, "FM", "b"), ("a_v", 1024, 512, "TM", "b"),
    ("b_f", 1536, 768, "FM", "f"), ("b_q", 2304, 768, "FM", "f"), ("b_i", 3072, 768, "TM", "b"),
    ("b_g", 3840, 768, "TM", "f"), ("c_q", 4608, 768, "FM", "b"), ("c_kc", 5376, 256, "FM", "b"),
    ("c_vc", 5632, 256, "FM", "b"), ("c_ks", 5888, 256, "FM", "b"), ("c_vs", 6144, 256, "TM", "b"),
    ("c_kw", 6400, 256, "FM", "b"), ("c_vw", 6656, 256, "TM", "b"), ("c_g", 6912, 18, "TM", "f"),
]
FMB_BASE = {"a_q": 0, "a_k": 4, "c_q": 8, "c_kc": 14, "c_vc": 16, "c_ks": 18, "c_kw": 20}
NFMB = 22
FMF_BASE = {"b_f": 0, "b_q": 6}
NFMF = 12
TMV_BASE = {"a_v": 0, "c_vs": 4, "c_vw": 6}
NTMV = 8
VP = 132

WSHAPES = [
    ("attn_norm", [D]), ("w_in", [D, DIN]), ("da_lam_q1", [64]), ("da_lam_k1", [64]),
    ("da_lam_q2", [64]), ("da_lam_k2", [64]), ("da_norm", [4, 128]), ("hg_norm", [6, 128]),
    ("nsa_pe_k", [32, 128]), ("nsa_pe_v", [32, 128]), ("nsa_ck_w1", [4096, 256]),
    ("nsa_ck_w2", [256, 128]), ("nsa_cv_w1", [4096, 256]), ("nsa_cv_w2", [256, 128]),
    ("w_out", [D, D]), ("ffn_norm", [D]), ("w_gate", [D, DFF]), ("w_up", [D, DFF]),
    ("w_down", [DFF, D]),
]


class _Op:
    __slots__ = ("eng", "fn", "deps", "sig", "sem", "val", "chan")


class Prog:
    ENGS = ("pe", "act", "dve", "pool", "sp")

    def __init__(self, nc):
        self.nc = nc
        self.ops = {e: [] for e in self.ENGS}
        self.res = {}
        self.stack = contextlib.ExitStack()
        self.last = {e: None for e in self.ENGS}
        self.pending_dma = []

    def sbuf(self, name, shape, dt):
        return self.stack.enter_context(self.nc.sbuf_tensor(name, list(shape), dt))

    def psum(self, name, shape, dt):
        return self.stack.enter_context(self.nc.psum_tensor(name, list(shape), dt))

    def op(self, eng, fn, reads=(), writes=(), chan=None):
        o = _Op()
        o.eng = eng
        o.fn = fn
        o.chan = chan
        o.sig = chan is not None
        o.sem = None
        o.val = 0
        deps = []
        for r in reads:
            st = self.res.get(r)
            if st is not None and st[0] is not None:
                deps.append(st[0])
        for w in writes:
            st = self.res.get(w)
            if st is not None:
                if st[0] is not None:
                    deps.append(st[0])
                deps.extend(st[1])
        seen = set()
        dd = []
        for d in deps:
            if id(d) in seen or d is o:
                continue
            seen.add(id(d))
            dd.append(d)
        o.deps = dd
        self.ops[eng].append(o)
        for r in reads:
            st = self.res.get(r)
            if st is None:
                self.res[r] = [None, [o]]
            else:
                st[1].append(o)
        for w in writes:
            self.res[w] = [o, []]
        if fn is not None:
            self.last[eng] = o
        if chan is not None:
            self.pending_dma.append(o)
        return o

    def dma(self, eng, out, in_, reads=(), writes=(), chan=None, **kw):
        assert chan is not None
        return self.op(eng, lambda e: e.dma_start(out=out, in_=in_, **kw),
                       reads=reads, writes=writes, chan=chan)

    def barrier(self):
        lasts = [o for o in self.last.values() if o is not None] + list(self.pending_dma)
        self.pending_dma = []
        for e in self.ENGS:
            o = self.op(e, None)
            o.deps = list(lasts)
        self.res = {}

    def emit(self):
        nc = self.nc
        for e in self.ENGS:
            for o in self.ops[e]:
                for d in o.deps:
                    if d.chan is None:
                        if d.eng == "pe" and o.eng == "pe":
                            continue
                        d.sig = True
        sems = {}
        for e in self.ENGS:
            sems[e] = self.stack.enter_context(nc.semaphore("sem_" + e))
        chans = sorted({o.chan for e in self.ENGS for o in self.ops[e] if o.chan is not None})
        for c in chans:
            sems["c:" + c] = self.stack.enter_context(nc.semaphore("semc_" + c.replace(".", "_")))
        cnt = {}
        for e in self.ENGS:
            n = 0
            for o in self.ops[e]:
                if o.chan is not None:
                    k = "c:" + o.chan
                    cnt[k] = cnt.get(k, 0) + 16
                    o.sem = sems[k]
                    o.val = cnt[k]
                elif o.sig:
                    assert o.fn is not None
                    n += 1
                    o.sem = sems[e]
                    o.val = n
        stats = {}

        def run(ename, eng):
            waited = {}
            nw = 0
            for o in self.ops[ename]:
                need = {}
                for d in o.deps:
                    if d.sem is None:
                        continue
                    if d.chan is None and d.eng == ename and ename == "pe":
                        continue
                    key = id(d.sem)
                    if waited.get(key, 0) >= d.val:
                        continue
                    if key not in need or need[key][1] < d.val:
                        need[key] = (d.sem, d.val)
                for key, (sem, val) in need.items():
                    eng.wait_ge(sem, val)
                    waited[key] = val
                    nw += 1
                if o.fn is None:
                    continue
                inst = o.fn(eng)
                if o.sem is not None:
                    inst.then_inc(o.sem, 16 if o.chan is not None else 1)
            stats[ename] = (len(self.ops[ename]), nw)

        with nc.Block() as block:
            @block.sync
            def _(e):
                run("sp", e)

            @block.tensor
            def _(e):
                run("pe", e)

            @block.scalar
            def _(e):
                run("act", e)

            @block.vector
            def _(e):
                run("dve", e)

            @block.gpsimd
            def _(e):
                run("pool", e)
        self.stats = stats
        self.stack.close()


class Arena:
    def __init__(self, tf, tb, nf, nb):
        self.tf, self.tb, self.nf, self.nb = tf, tb, nf, nb
        self.of = self.ob = 0

    def reset(self):
        self.of = self.ob = 0

    @staticmethod
    def _shape(ap, shape):
        if len(shape) == 2:
            return ap
        if len(shape) == 3:
            return ap.rearrange("p (a b) -> p a b", a=shape[1])
        raise ValueError

    def f32(self, shape):
        n = int(np.prod(shape[1:]))
        n4 = (n + 3) // 4 * 4
        assert self.of + n4 <= self.nf, ("f32 arena overflow", self.of, n4, self.nf)
        ap = self.tf[0:shape[0], self.of:self.of + n]
        self.of += n4
        return self._shape(ap, shape)

    def bf(self, shape):
        n = int(np.prod(shape[1:]))
        n8 = (n + 7) // 8 * 8
        assert self.ob + n8 <= self.nb, ("bf16 arena overflow", self.ob, n8, self.nb)
        ap = self.tb[0:shape[0], self.ob:self.ob + n]
        self.ob += n8
        return self._shape(ap, shape)


def host_consts():
    bf = ml_dtypes.bfloat16
    p = np.arange(128)[:, None]
    j = np.arange(128)[None, :]
    ident = (p == j).astype(np.float32)
    tri = (j >= p).astype(np.float32)
    negc = np.where(j < p, NEG, 0.0).astype(np.float32)
    negw = np.where(p <= j, NEG, 0.0).astype(np.float32)
    cb = np.stack([ident, tri, negc, negw], axis=1).astype(bf)
    n = np.arange(127)[:, None]
    t = np.arange(T)[None, :]
    cmask = (16 * n + 31 <= t).astype(np.float32)
    cmask = np.concatenate([cmask, np.zeros((1, T), np.float32)], 0).astype(bf)
    c_start = np.arange(127) * 16
    s_start = np.arange(32) * 64
    ov = ((c_start[:, None] < s_start[None, :] + 64) & (c_start[:, None] + 32 > s_start[None, :])).astype(np.float32)
    ov = np.concatenate([ov, np.zeros((1, 32), np.float32)], 0).astype(bf)
    tt = np.arange(T)[:, None]
    blk = np.arange(32)[None, :]
    cur = tt // 64
    forced = (blk == 0) | (blk == cur) | (blk == cur - 1)
    valid = blk * 64 <= tt
    sb = np.where(valid, np.where(forced, 1.0e4, 0.0), -1.0e30).astype(np.float32)
    selbias = np.ascontiguousarray(sb.reshape(NT, 128, 32).transpose(1, 0, 2))
    key = np.arange(T)[None, :]
    expand = np.where(np.arange(32)[:, None] == key // 64, NEG, 0.0).astype(bf)
    tq = np.arange(T).reshape(NT, 128).T
    phz = np.zeros((128, NT, 2), np.float32)
    phz[:, :, 1] = np.maximum(0, 511 - tq)
    return {"c_b": cb, "c_cmask": cmask, "c_ov": ov, "c_selbias": selbias, "c_expand": expand, "c_phz": phz,
            "c_identf": np.eye(32, dtype=np.float32)}


def layer_consts(layers):
    lc = np.zeros((len(layers), 128, 8), np.float32)
    for i, l in enumerate(layers):
        lam_init = 0.8 - 0.6 * math.exp(-0.3 * l)
        lc[i, :, 0] = lam_init
        lc[i, :, 1] = 1.0 - lam_init
        for m in range(4):
            lc[i, :, 4 + m] = 1.0 if (1 <= m <= l) else 0.0
    return lc


class Cfg:
    def __init__(self, nl=1, final=False, phases=("pre", "A", "B", "C", "post"), scratch_in=False,
                 taps=(), lim=None):
        self.lim = lim or {}
        self.nl = nl
        self.final = final
        self.phases = phases
        self.scratch_in = scratch_in
        self.taps = taps


def build(cfg):
    nc = bass.Bass("TRN2", target_bir_lowering=False)
    P = Prog(nc)
    NL = cfg.nl

    def dram(name, shape, dt, kind="Internal"):
        return nc.dram_tensor(name, list(shape), dt, kind=kind).ap()

    x_in = dram("x", [T, D], F32, "ExternalInput")
    out = dram("out", [T, D], F32, "ExternalOutput")
    W = {n: dram(n, [NL] + s, F32, "ExternalInput") for n, s in WSHAPES}
    hg_gamma = dram("hg_gamma", [4, 768], F32, "ExternalInput")
    final_norm = dram("final_norm", [D], F32, "ExternalInput")
    lconst_d = dram("lconst", [NL, 128, 8], F32, "ExternalInput")
    c_b_d = dram("c_b", [128, 4, 128], BF16, "ExternalInput")
    c_cmask_d = dram("c_cmask", [128, T], BF16, "ExternalInput")
    c_ov_d = dram("c_ov", [128, 32], BF16, "ExternalInput")
    c_selbias_d = dram("c_selbias", [128, NT, 32], F32, "ExternalInput")
    c_expand_d = dram("c_expand", [32, T], BF16, "ExternalInput")
    c_phz_d = dram("c_phz", [128, NT, 2], F32, "ExternalInput")
    c_identf_d = dram("c_identf", [32, 32], F32, "ExternalInput")

    sk = "ExternalInput" if cfg.scratch_in else ("ExternalOutput" if "scratch" in cfg.taps else "Internal")
    fmb = dram("fmb", [NFMB, 128, T], BF16, sk)
    fmf = dram("fmf", [NFMF, 128, T], F32, sk)
    tmv = dram("tmv", [NTMV, 128, NT, VP], BF16, sk)
    tmbi = dram("tmbi", [6, 64, 32, 128], BF16, sk)
    tmbg = dram("tmbg", [6, 64, 32, 128], F32, sk)
    tmg = dram("tmg", [2, 128, NT, 9], F32, sk)
    xmid = dram("xmid", [T, D], F32, "ExternalOutput" if "xmid" in cfg.taps else "Internal")
    xbuf = [dram("xbuf%d" % i, [T, D], F32) for i in range(2)]
    yT_dbg = dram("yT_dbg", [128, 16, T], BF16, "ExternalOutput") if "yT" in cfg.taps else None
    yT_in = dram("yT_in", [128, 16, T], BF16, "ExternalInput") if "yT_in" in cfg.taps else None

    actT = P.sbuf("actT", [128, 16 * T], BF16)
    NF_AR = 14336
    NB_AR = 38912
    ar_f = P.sbuf("ar_f", [128, NF_AR], F32)
    ar_b = P.sbuf("ar_b", [128, NB_AR], BF16)
    AR = Arena(ar_f, ar_b, NF_AR, NB_AR)
    cb = P.sbuf("cb", [128, 4, 128], BF16)
    ident = cb[:, 0, :]
    tri = cb[:, 1, :]
    negc = cb[:, 2, :]
    negw = cb[:, 3, :]
    lconst = P.sbuf("lconst_sb", [128, NL * 8], F32)
    lbt = P.sbuf("lbt", [128, 6 * 4], F32)
    lbs = P.sbuf("lbs", [128, 8], F32)
    smallf = P.sbuf("smallf", [128, 384], F32)
    gB_sb = P.sbuf("gB_sb", [128, 768], F32)
    identf = P.sbuf("identf", [32, 32], F32)
    pf = [P.psum("pf%d" % i, [128, 512], F32) for i in range(8)]
    epsb = P.sbuf("epsb", [128, 1], F32)
    P.op("dve", lambda e: e.memset(epsb[:], EPS), writes=["epsb"])

    def pbf(i, c0=0, n=1024):
        return pf[i][:, :].bitcast(BF16)[:, c0:c0 + n]

    hT3 = actT[:, :].rearrange("p (c t) -> p c t", c=16)

    P.dma("sp", cb[:], c_b_d[:, :, :], writes=["cb"], chan="cb")
    phz = P.sbuf("phz", [128, NT, 2], F32)
    P.dma("sp", phz[:], c_phz_d[:, :, :], writes=["phz"], chan="phz")
    P.dma("sp", lconst[:].rearrange("p (l k) -> p l k", l=NL), lconst_d.rearrange("l p k -> p l k"),
          writes=["lconst"], chan="lconst")
    P.dma("sp", identf[:], c_identf_d[:, :], writes=["identf"], chan="identf")
    hg_sb = P.sbuf("hg_sb", [24, 128], F32)
    P.dma("sp", hg_sb[:], hg_gamma.rearrange("l (h p) -> (l h) p", p=128), writes=["hg_sb"], chan="hg_sb")
    P.op("pe", lambda e: e.matmul(pf[0][:, 0:24], lhsT=hg_sb[:], rhs=identf[0:24, 0:24], start=True, stop=True),
         reads=["hg_sb", "identf"], writes=["pf0"])
    P.op("dve", lambda e: e.tensor_copy(out=lbt[:], in_=pf[0][:, 0:24]), reads=["pf0"], writes=["lbt"])
    P.op("act", lambda e: e.activation(out=lbt[:], in_=lbt[:], func=AF.Exp), reads=["lbt"], writes=["lbt"])
    P.op("dve", lambda e: e.tensor_reduce(out=lbs[:, 0:6], in_=lbt[:].rearrange("p (l h) -> p h l", l=4),
                                          axis=AX.X, op=ALU.add), reads=["lbt"], writes=["lbs"])
    P.op("dve", lambda e: e.reciprocal(out=lbs[:, 0:6], in_=lbs[:, 0:6]), reads=["lbs"], writes=["lbs"])

    evac_rr = [0]

    def evac(out_ap, in_ap, reads, writes):
        evac_rr[0] += 1
        if evac_rr[0] % 2:
            return P.op("act", lambda e: e.copy(out=out_ap, in_=in_ap), reads=reads, writes=writes)
        return P.op("dve", lambda e: e.tensor_copy(out=out_ap, in_=in_ap), reads=reads, writes=writes)

    def rstd_ops(ss_ap, n, res):
        P.op("act", lambda e: e.activation(out=ss_ap, in_=ss_ap, func=AF.Ln, bias=epsb[0:ss_ap.shape[0], :], scale=1.0 / n),
             reads=[res], writes=[res])
        P.op("act", lambda e: e.activation(out=ss_ap, in_=ss_ap, func=AF.Exp, scale=-0.5), reads=[res], writes=[res])

    def norm_to_T(tag, xsrc, gvec, dstT, tok0, ntile, gbc, xt, hn, ssb):
        for i in range(ntile):
            s = i % 2
            P.dma("sp", xt[s], xsrc[i * 128:(i + 1) * 128, :], writes=[tag + "xt%d" % s], chan=tag + "xt%d" % s)
            P.op("act", lambda e, s=s, i=i: e.activation(out=hn[s], in_=xt[s], func=AF.Square,
                                                         accum_out=ssb[:, i:i + 1]),
                 reads=[tag + "xt%d" % s], writes=[tag + "hn%d" % s, tag + "ss%d" % i])
            rstd_ops(ssb[:, i:i + 1], D, tag + "ss%d" % i)
            P.op("dve", lambda e, s=s, i=i: e.scalar_tensor_tensor(out=hn[s], in0=xt[s], scalar=ssb[:, i:i + 1],
                                                                   in1=gbc, op0=ALU.mult, op1=ALU.mult),
                 reads=[tag + "xt%d" % s, tag + "ss%d" % i, tag + "gbc"], writes=[tag + "hn%d" % s])
            for g in range(4):
                bank = 6 + (g % 2)
                for k in range(4):
                    c = g * 4 + k
                    P.op("pe", lambda e, s=s, c=c, k=k, bank=bank: e.transpose(
                        out=pbf(bank, k * 128, 128), in_=hn[s][:, c * 128:(c + 1) * 128], identity=ident),
                        reads=[tag + "hn%d" % s, "cb"], writes=["pf%d" % bank])
                t0 = tok0 + i * 128
                evac(dstT[:, g * 4:(g + 1) * 4, t0:t0 + 128],
                     pbf(bank, 0, 512).rearrange("p (k t) -> p k t", k=4),
                     reads=["pf%d" % bank], writes=[tag + "dstT"])

    def phase_pre(li, xsrc):
        AR.reset()
        tag = "pre."
        xt = [AR.f32([128, D]) for _ in range(2)]
        gbc = AR.f32([128, D])
        fst = [AR.f32([128, T]) for _ in range(2)]
        hn = [AR.bf([128, D]) for _ in range(2)]
        wb = [AR.bf([128, 16, 512]) for _ in range(3)]
        bst = [AR.bf([128, T]) for _ in range(2)]
        tst_b = [AR.bf([128, 512]) for _ in range(2)]
        tst_f = [AR.f32([128, 512]) for _ in range(2)]
        tst_v = [AR.bf([128, 4, VP]) for _ in range(2)]
        for i_ in range(2):
            P.op("pool", lambda e, i_=i_: e.memset(tst_v[i_][:, :, 128:VP], 0.0), writes=[tag + "tstv%d" % i_])
            P.op("pool", lambda e, i_=i_: e.memset(tst_v[i_][:, :, 128:129], 1.0), reads=[tag + "tstv%d" % i_], writes=[tag + "tstv%d" % i_])
        tvi = 0
        ssb = smallf[:, 0:16]
        P.op("dve", lambda e: e.memset(ssb, 0.0), writes=[tag + "ss%d" % i for i in range(16)])
        P.dma("sp", gbc, W["attn_norm"][li].partition_broadcast(128), writes=[tag + "gbc"], chan=tag + "gbc")
        norm_to_T(tag, xsrc, None, hT3, 0, NT, gbc, xt, hn, ssb)
        wv = W["w_in"][li].rearrange("(c p) n -> p c n", p=128)
        pi = 0
        fmi = 0
        tmi = 0
        bank_rr = 0
        for (name, c0, ncol, kind, dt) in SEG:
            off = 0
            while off < ncol:
                nco = min(512, ncol - off)
                s = pi % 3
                pi += 1
                wres = tag + "wb%d" % s
                P.dma("pool", wb[s][:, :, 0:nco], wv[:, :, c0 + off:c0 + off + nco], writes=[wres], chan=wres)
                if kind == "FM":
                    for jt in range(nco // 128):
                        tile_id = (off // 128) + jt
                        if dt == "b":
                            st = bst[fmi % 2]
                            stres = tag + "bst%d" % (fmi % 2)
                            dst = fmb[FMB_BASE[name] + tile_id]
                        else:
                            st = fst[fmi % 2]
                            stres = tag + "fst%d" % (fmi % 2)
                            dst = fmf[FMF_BASE[name] + tile_id]
                        fmi += 1
                        for tb in range(T // 512):
                            bank = bank_rr % 4
                            bank_rr += 1
                            for c in range(16):
                                P.op("pe", lambda e, s=s, c=c, jt=jt, tb=tb, bank=bank: e.matmul(
                                    pf[bank][:, :], lhsT=wb[s][:, c, jt * 128:(jt + 1) * 128],
                                    rhs=hT3[:, c, tb * 512:(tb + 1) * 512], start=(c == 0), stop=(c == 15)),
                                    reads=[wres, tag + "dstT"], writes=["pf%d" % bank])
                            evac(st[:, tb * 512:(tb + 1) * 512], pf[bank][:, :], reads=["pf%d" % bank], writes=[stres])
                        P.dma("sp", dst, st, reads=[stres], chan=stres)
                else:
                    for it in range(NT):
                        bank = bank_rr % 4
                        bank_rr += 1
                        for c in range(16):
                            P.op("pe", lambda e, s=s, c=c, it=it, bank=bank, nco=nco: e.matmul(
                                pf[bank][:, 0:nco], lhsT=hT3[:, c, it * 128:(it + 1) * 128],
                                rhs=wb[s][:, c, 0:nco], start=(c == 0), stop=(c == 15)),
                                reads=[wres, tag + "dstT"], writes=["pf%d" % bank])
                        nj = nco // 128
                        j0 = off // 128
                        if name in TMV_BASE:
                            st = tst_v[tvi % 2]
                            stres = tag + "tstv%d" % (tvi % 2)
                            tvi += 1
                            evac(st[:, 0:nj, 0:128], pf[bank][:, 0:nco].rearrange("p (j c) -> p j c", c=128),
                                 reads=["pf%d" % bank], writes=[stres])
                            t0_ = TMV_BASE[name] + j0
                            P.dma("sp", tmv[t0_:t0_ + nj, :, it, :].rearrange("j p c -> p j c"), st[:, 0:nj, :],
                                  reads=[stres], chan=stres)
                            continue
                        if dt == "b":
                            st = tst_b[tmi % 2]
                            stres = tag + "tstb%d" % (tmi % 2)
                        else:
                            st = tst_f[tmi % 2]
                            stres = tag + "tstf%d" % (tmi % 2)
                        tmi += 1
                        evac(st[:, 0:nco], pf[bank][:, 0:nco], reads=["pf%d" % bank], writes=[stres])
                        if False:
                            pass
                        elif name in ("b_i", "b_g"):
                            dd = tmbi if name == "b_i" else tmbg
                            for half in range(2):
                                P.dma("sp", dd[j0:j0 + nj, :, 2 * it + half, :].rearrange("j p c -> p j c"),
                                      st[64 * half:64 * half + 64, 0:nco].rearrange("p (j c) -> p j c", c=128),
                                      reads=[stres], chan=stres)
                        else:
                            for g_ in range(2):
                                P.dma("sp", tmg[g_, :, it, :], st[:, 9 * g_:9 * g_ + 9], reads=[stres], chan=stres)
                off += nco
        P.barrier()

    def yT_store(chunk, t0, n, in_ap, scale_ap, in_res):
        o_ap = hT3[:, chunk, t0:t0 + n]
        if scale_ap is None:
            P.op("act", lambda e: e.copy(out=o_ap, in_=in_ap), reads=in_res, writes=["yT.%d" % chunk])
        else:
            P.op("act", lambda e: e.activation(out=o_ap, in_=in_ap, func=AF.Copy, scale=scale_ap),
                 reads=in_res, writes=["yT.%d" % chunk])

    def phase_A(li):
        AR.reset()
        tag = "A."
        qT = [AR.bf([128, T]) for _ in range(2)]
        kT = [AR.bf([128, T]) for _ in range(2)]
        va = [AR.bf([128, NT, VP]) for _ in range(2)]
        PT = [AR.bf([128, 512]) for _ in range(4)]
        onb = [AR.bf([128, 128]) for _ in range(2)]
        lamb = AR.f32([128, 4, 64])
        lamt = AR.f32([128, 2, 64])
        osb = [AR.f32([128, 128]) for _ in range(2)]
        t1 = [AR.f32([128, 128]) for _ in range(2)]
        sqj = AR.f32([128, 128])
        sm = smallf
        lam = sm[:, 0:1]
        nlam = sm[:, 1:2]
        s12 = sm[:, 2:4]
        gA = sm[:, 4:8]
        ss2 = sm[:, 16:80]
        r2 = sm[:, 80:208]
        rl = sm[:, 208:216]
        lc = lconst[:, li * 8:(li + 1) * 8]
        for k, nm in enumerate(["da_lam_q1", "da_lam_k1", "da_lam_q2", "da_lam_k2"]):
            P.dma("sp", lamb[:, k, :], W[nm][li].partition_broadcast(128), writes=[tag + "lamb%d" % k], chan=tag + "lamb%d" % k)
        P.op("dve", lambda e: e.tensor_tensor(out=lamt[:, 0, :], in0=lamb[:, 0, :], in1=lamb[:, 1, :], op=ALU.mult),
             reads=[tag + "lamb0", tag + "lamb1"], writes=[tag + "lamt"])
        P.op("dve", lambda e: e.tensor_tensor(out=lamt[:, 1, :], in0=lamb[:, 2, :], in1=lamb[:, 3, :], op=ALU.mult),
             reads=[tag + "lamb2", tag + "lamb3", tag + "lamt"], writes=[tag + "lamt"])
        P.op("dve", lambda e: e.tensor_reduce(out=s12, in_=lamt[:, :, :], axis=AX.X, op=ALU.add),
             reads=[tag + "lamt"], writes=[tag + "s12"])
        P.op("act", lambda e: e.activation(out=s12, in_=s12, func=AF.Exp), reads=[tag + "s12"], writes=[tag + "s12"])
        P.op("dve", lambda e: e.tensor_tensor(out=lam, in0=sm[:, 2:3], in1=sm[:, 3:4], op=ALU.subtract),
             reads=[tag + "s12"], writes=[tag + "lam"])
        P.op("dve", lambda e: e.tensor_scalar(out=lam, in0=lam, scalar1=lc[:, 0:1], scalar2=None, op0=ALU.add),
             reads=[tag + "lam", "lconst"], writes=[tag + "lam"])
        P.op("dve", lambda e: e.tensor_scalar(out=nlam, in0=lam, scalar1=-1.0, scalar2=None, op0=ALU.mult),
             reads=[tag + "lam"], writes=[tag + "nlam"])
        gArow = AR.f32([128, 512])
        P.dma("sp", gArow, W["da_norm"][li].rearrange("h d -> (h d)").partition_broadcast(128), writes=[tag + "gA"], chan=tag + "gA")
        P.op("dve", lambda e: e.tensor_scalar(out=gArow, in0=gArow, scalar1=lc[:, 1:2], scalar2=None, op0=ALU.mult),
             reads=[tag + "gA", "lconst"], writes=[tag + "gA"])
        P.op("dve", lambda e: e.memset(ss2, 0.0), writes=[tag + "ss2"])
        grp = 0
        for h in range(cfg.lim.get("A_heads", 4)):
            hb = h % 2
            qres, kres, vres = tag + "qT%d" % hb, tag + "kT%d" % hb, tag + "va%d" % hb
            P.dma("sp", qT[hb], fmb[FMB_BASE["a_q"] + h], reads=["scr.a_q"], writes=[qres], chan=qres)
            P.dma("sp", kT[hb], fmb[FMB_BASE["a_k"] + h], reads=["scr.a_k"], writes=[kres], chan=kres)
            P.dma("sp", va[hb], tmv[TMV_BASE["a_v"] + h], writes=[vres], chan=vres)
            for i in range(cfg.lim.get("A_tiles", NT)):
                ob = 2 + (i % 2)
                for c in range(2):
                    oc = pf[ob][:, c * 256:c * 256 + 129]
                    for g0 in range(0, i + 1, 4):
                        kts = list(range(g0, min(g0 + 4, i + 1)))
                        sb = grp % 2
                        ps = grp % 4
                        grp += 1
                        for jj, kt in enumerate(kts):
                            P.op("pe", lambda e, c=c, kt=kt, jj=jj, i=i, sb=sb, hb=hb: e.matmul(
                                pf[sb][:, jj * 128:(jj + 1) * 128],
                                lhsT=kT[hb][64 * c:64 * c + 64, kt * 128:(kt + 1) * 128],
                                rhs=qT[hb][64 * c:64 * c + 64, i * 128:(i + 1) * 128], start=True, stop=True),
                                reads=[qres, kres], writes=["pf%d" % sb])
                        n = len(kts) * 128
                        P.op("act", lambda e, sb=sb, ps=ps, n=n: e.activation(out=PT[ps][:, 0:n], in_=pf[sb][:, 0:n],
                                                                                func=AF.Exp, scale=0.125),
                             reads=["pf%d" % sb], writes=[tag + "PT%d" % ps])
                        if kts[-1] == i:
                            jj = len(kts) - 1
                            P.op("pool", lambda e, ps=ps, jj=jj: e.tensor_tensor(
                                out=PT[ps][:, jj * 128:(jj + 1) * 128], in0=PT[ps][:, jj * 128:(jj + 1) * 128],
                                in1=tri, op=ALU.mult), reads=[tag + "PT%d" % ps, "cb"], writes=[tag + "PT%d" % ps])
                        for jj, kt in enumerate(kts):
                            P.op("pe", lambda e, ps=ps, jj=jj, kt=kt, i=i, oc=oc, hb=hb: e.matmul(
                                oc, lhsT=PT[ps][:, jj * 128:(jj + 1) * 128], rhs=va[hb][:, kt, 0:129],
                                start=(kt == 0), stop=(kt == i)),
                                reads=[tag + "PT%d" % ps, vres], writes=["pf%d" % ob])
                u = h * NT + i
                ores = "pf%d" % ob
                s = i % 2
                rr = r2[:, 2 * u:2 * u + 2]
                P.op("dve", lambda e, ob=ob, rr=rr: e.reciprocal(out=rr, in_=pf[ob][:, 128:512:256]),
                     reads=[ores], writes=[tag + "r%d" % u])
                P.op("dve", lambda e, rr=rr, s=s: e.tensor_tensor(out=rl[:, s:s + 1], in0=rr[:, 1:2], in1=nlam, op=ALU.mult),
                     reads=[tag + "r%d" % u, tag + "nlam"], writes=[tag + "rl%d" % s])
                P.op("dve", lambda e, ob=ob, s=s: e.tensor_scalar(out=t1[s], in0=pf[ob][:, 256:384], scalar1=rl[:, s:s + 1],
                                                                  scalar2=None, op0=ALU.mult),
                     reads=[ores, tag + "rl%d" % s], writes=[tag + "t1%d" % s])
                P.op("dve", lambda e, ob=ob, s=s, rr=rr: e.scalar_tensor_tensor(
                    out=osb[s], in0=pf[ob][:, 0:128], scalar=rr[:, 0:1], in1=t1[s], op0=ALU.mult, op1=ALU.add),
                    reads=[ores, tag + "r%d" % u, tag + "t1%d" % s], writes=[tag + "osb%d" % s])
                P.op("act", lambda e, s=s, u=u: e.activation(out=sqj, in_=osb[s], func=AF.Square, accum_out=ss2[:, u:u + 1]),
                     reads=[tag + "osb%d" % s, tag + "ss2"], writes=[tag + "sqj", tag + "ssu%d" % u])
                rstd_ops(ss2[:, u:u + 1], 128, tag + "ssu%d" % u)
                P.op("dve", lambda e, s=s, u=u, h=h: e.scalar_tensor_tensor(out=onb[s], in0=osb[s], scalar=ss2[:, u:u + 1],
                                                                       in1=gArow[:, h * 128:(h + 1) * 128], op0=ALU.mult, op1=ALU.mult),
                     reads=[tag + "osb%d" % s, tag + "ssu%d" % u, tag + "gA"], writes=[tag + "onb%d" % s])
                tb = 6 + (i % 2)
                P.op("pe", lambda e, s=s, tb=tb: e.transpose(out=pbf(tb, 0, 128), in_=onb[s], identity=ident),
                     reads=[tag + "onb%d" % s, "cb"], writes=["pf%d" % tb])
                yT_store(h, i * 128, 128, pbf(tb, 0, 128), None, ["pf%d" % tb])
        P.barrier()

    def phase_B(li):
        tag = "B."
        lc = lconst[:, li * 8:(li + 1) * 8]
        C = 64
        NCH = T // C
        sm = smallf
        gBrow = gB_sb[0:64, :]
        lbv = sm[:, 6:12]
        oml = sm[:, 12:13]

        def sc(k, j):
            b0 = 16 + k * 96 + j * 32
            return sm[:, b0:b0 + 32]

        for hbatch in range(cfg.lim.get("B_batches", 3)):
            AR.reset()
            heads = [hbatch * 2 + k for k in range(2)]
            NH = len(heads)
            zt = AR.f32([128, T])
            lf = AR.f32([128, T])
            cu = AR.f32([128, T])
            msk = AR.f32([128, T])
            gtmp = AR.f32([64, NCH * 128])
            Sst = [AR.f32([128, 128]) for _ in range(NH)]
            tmp2 = [[AR.f32([128, 128]) for _ in range(2)] for _ in range(NH)]
            ogf = [AR.f32([64, 128]) for _ in range(NH)]
            sqj = AR.f32([64, 128])
            ssb = [AR.f32([64, NCH]) for _ in range(NH)]
            ky = AR.bf([128, T])
            qe = [AR.bf([128, T]) for _ in range(NH)]
            ke = [AR.bf([128, T]) for _ in range(NH)]
            vv = [AR.bf([64, NCH, 128]) for _ in range(NH)]
            sg = [AR.bf([64, NCH, 128]) for _ in range(NH)]
            kd = [[AR.bf([64, 128]) for _ in range(2)] for _ in range(NH)]
            ATb = [[AR.bf([64, 64]) for _ in range(2)] for _ in range(NH)]
            Sm = [[AR.bf([128, 128]) for _ in range(2)] for _ in range(NH)]
            ybf = [[AR.bf([64, 128]) for _ in range(2)] for _ in range(NH)]
            if hbatch == 0:
                P.dma("sp", gBrow, W["hg_norm"][li].rearrange("h d -> (h d)").partition_broadcast(64), writes=[tag + "gB"], chan=tag + "gB")
                lb3 = lbt[:].rearrange("p (l h) -> p l h", l=4)
                P.op("dve", lambda e: e.tensor_scalar(out=lbv, in0=lb3[:, 0, :], scalar1=lc[:, 4:5], scalar2=None, op0=ALU.mult),
                     writes=[tag + "lbv"])
                for m in range(1, 4):
                    P.op("dve", lambda e, m=m: e.scalar_tensor_tensor(out=lbv, in0=lb3[:, m, :], scalar=lc[:, 4 + m:5 + m],
                                                                      in1=lbv, op0=ALU.mult, op1=ALU.add),
                         reads=[tag + "lbv"], writes=[tag + "lbv"])
                P.op("dve", lambda e: e.tensor_tensor(out=lbv, in0=lbv, in1=lbs[:, 0:6], op=ALU.mult),
                     reads=[tag + "lbv"], writes=[tag + "lbv"])
            P.op("pool", lambda e: e.memset(msk, 1.0), writes=[tag + "msk"])
            P.op("pool", lambda e: e.memset(msk.rearrange("p (n c) -> p n c", c=C)[:, :, 0:1], 0.0),
                 reads=[tag + "msk"], writes=[tag + "msk"])
            for k, h in enumerate(heads):
                hk = tag + "h%d." % k
                P.dma("sp", zt, fmf[FMF_BASE["b_f"] + h], writes=[tag + "zt"], chan=tag + "zt")
                P.dma("sp", vv[k], tmbi[h], writes=[hk + "vv"], chan=hk + "vv")
                P.dma("sp", gtmp.rearrange("p (n c) -> p n c", c=128), tmbg[h], writes=[tag + "gtmp"], chan=tag + "gtmp")
                P.op("act", lambda e, k=k: e.activation(out=sg[k][:, :, :].rearrange("p n c -> p (n c)"), in_=gtmp, func=AF.Silu),
                     reads=[tag + "gtmp"], writes=[hk + "sg"])
                P.op("act", lambda e: e.activation(out=zt, in_=zt, func=AF.Sigmoid), reads=[tag + "zt"], writes=[tag + "zt"])
                P.op("dve", lambda e, h=h: e.tensor_scalar(out=oml, in0=lbv[:, h:h + 1], scalar1=-1.0, scalar2=1.0,
                                                           op0=ALU.mult, op1=ALU.add),
                     reads=[tag + "lbv"], writes=[tag + "oml"])
                P.op("dve", lambda e, h=h: e.tensor_scalar(out=zt, in0=zt, scalar1=oml, scalar2=lbv[:, h:h + 1],
                                                           op0=ALU.mult, op1=ALU.add),
                     reads=[tag + "zt", tag + "oml", tag + "lbv"], writes=[tag + "zt"])
                P.op("act", lambda e: e.activation(out=lf, in_=zt, func=AF.Ln), reads=[tag + "zt"], writes=[tag + "lf"])
                P.op("pool", lambda e: e.tensor_scalar(out=ky, in0=zt, scalar1=-1.0, scalar2=1.0, op0=ALU.mult, op1=ALU.add),
                     reads=[tag + "zt"], writes=[tag + "ky"])
                P.op("dve", lambda e: e.tensor_tensor_scan(out=cu, data0=msk, data1=lf, initial=0.0, op0=ALU.mult, op1=ALU.add),
                     reads=[tag + "msk", tag + "lf"], writes=[tag + "cu"])
                cu3 = cu.rearrange("p (n c) -> p n c", c=C)
                emid, elast, elm = sc(k, 0), sc(k, 1), sc(k, 2)
                P.op("act", lambda e, emid=emid: e.activation(out=emid, in_=cu3[:, :, C // 2 - 1], func=AF.Exp),
                     reads=[tag + "cu"], writes=[hk + "emid"])
                P.op("act", lambda e, elast=elast: e.activation(out=elast, in_=cu3[:, :, C - 1], func=AF.Exp),
                     reads=[tag + "cu"], writes=[hk + "elast"])
                P.op("dve", lambda e, elm=elm: e.tensor_tensor(out=elm, in0=cu3[:, :, C - 1], in1=cu3[:, :, C // 2 - 1], op=ALU.subtract),
                     reads=[tag + "cu"], writes=[hk + "elm"])
                P.op("act", lambda e, elm=elm: e.activation(out=elm, in_=elm, func=AF.Exp), reads=[hk + "elm"], writes=[hk + "elm"])
                lf3 = lf.rearrange("p (n c) -> p n c", c=C)
                P.op("dve", lambda e: e.tensor_tensor(out=lf3, in0=cu3, in1=cu3[:, :, C // 2 - 1:C // 2].to_broadcast([128, NCH, C]),
                                                      op=ALU.subtract),
                     reads=[tag + "cu", tag + "lf"], writes=[tag + "lf"])
                P.dma("sp", zt, fmf[FMF_BASE["b_q"] + h], writes=[tag + "zt"], chan=tag + "zt")
                P.op("act", lambda e: e.activation(out=cu, in_=lf, func=AF.Exp), reads=[tag + "lf"], writes=[tag + "cu"])
                P.op("act", lambda e: e.activation(out=lf, in_=lf, func=AF.Exp, scale=-1.0), reads=[tag + "lf"], writes=[tag + "lf"])
                P.op("dve", lambda e, k=k: e.tensor_tensor(out=qe[k], in0=zt, in1=cu, op=ALU.mult),
                     reads=[tag + "zt", tag + "cu"], writes=[hk + "qe"])
                P.op("pool", lambda e, k=k: e.tensor_tensor(out=ke[k], in0=ky, in1=lf, op=ALU.mult),
                     reads=[tag + "ky", tag + "lf"], writes=[hk + "ke"])
                P.op("dve", lambda e, k=k: e.memset(Sst[k], 0.0), writes=[hk + "S"])
                P.op("dve", lambda e, k=k: e.memset(ssb[k], 0.0), writes=[hk + "ss%d" % n for n in range(NCH)])
            for n in range(cfg.lim.get("B_chunks", NCH)):
                par = n % 2
                for k, h in enumerate(heads):
                    hk = tag + "h%d." % k
                    emid, elast, elm = sc(k, 0), sc(k, 1), sc(k, 2)
                    bA = k
                    bS = 2 + k
                    bO = 4 + k
                    cs = slice(n * C, (n + 1) * C)
                    tb = 6 + ((n * NH + k) % 2)
                    P.op("pe", lambda e, k=k, cs=cs, tb=tb: e.transpose(out=pbf(tb, 0, 128)[0:64, :], in_=ke[k][:, cs], identity=ident),
                         reads=[hk + "ke"], writes=["pf%d" % tb])
                    evac(kd[k][par], pbf(tb, 0, 128)[0:64, :], reads=["pf%d" % tb], writes=[hk + "kd%d" % par])
                    P.op("pe", lambda e, k=k, cs=cs, bA=bA: e.matmul(pf[bA][0:64, 0:64], lhsT=ke[k][:, cs], rhs=qe[k][:, cs],
                                                                    start=True, stop=True),
                         reads=[hk + "ke", hk + "qe"], writes=["pf%d" % bA])
                    P.op("dve", lambda e, k=k, par=par, bA=bA: e.tensor_tensor(out=ATb[k][par], in0=pf[bA][0:64, 0:64],
                                                                               in1=tri[0:64, 0:64], op=ALU.mult),
                         reads=["pf%d" % bA], writes=[hk + "AT%d" % par])
                    oc = pf[bO][0:64, 0:128]
                    ores = "pf%d" % bO
                    if n > 0:
                        P.op("act", lambda e, k=k, par=par, n=n, emid=emid: e.activation(out=Sm[k][par], in_=Sst[k], func=AF.Copy,
                                                                                         scale=emid[:, n:n + 1]),
                             reads=[hk + "S", hk + "emid"], writes=[hk + "Sm%d" % par])
                    P.op("pe", lambda e, k=k, par=par, n=n, oc=oc: e.matmul(oc, lhsT=ATb[k][par], rhs=vv[k][:, n, :],
                                                                            start=True, stop=(n == 0)),
                         reads=[hk + "AT%d" % par, hk + "vv"], writes=[ores])
                    if n > 0:
                        P.op("pe", lambda e, k=k, par=par, cs=cs, oc=oc: e.matmul(oc, lhsT=qe[k][:, cs], rhs=Sm[k][par],
                                                                                  start=False, stop=True),
                             reads=[hk + "qe", hk + "Sm%d" % par], writes=[ores])
                    if n < NCH - 1:
                        P.op("pe", lambda e, k=k, par=par, n=n, bS=bS: e.matmul(pf[bS][:, 0:128], lhsT=kd[k][par], rhs=vv[k][:, n, :],
                                                                               start=True, stop=True),
                             reads=[hk + "kd%d" % par, hk + "vv"], writes=["pf%d" % bS])
                        P.op("dve", lambda e, k=k, par=par, n=n, bS=bS, elm=elm: e.tensor_scalar(
                            out=tmp2[k][par], in0=pf[bS][:, 0:128], scalar1=elm[:, n:n + 1], scalar2=None, op0=ALU.mult),
                            reads=["pf%d" % bS, hk + "elm"], writes=[hk + "tmp2%d" % par])
                        P.op("dve", lambda e, k=k, par=par, n=n, elast=elast: e.scalar_tensor_tensor(
                            out=Sst[k], in0=Sst[k], scalar=elast[:, n:n + 1], in1=tmp2[k][par], op0=ALU.mult, op1=ALU.add),
                            reads=[hk + "S", hk + "elast", hk + "tmp2%d" % par], writes=[hk + "S"])
                    P.op("act", lambda e, k=k, n=n, oc=oc: e.activation(out=sqj, in_=oc, func=AF.Square, accum_out=ssb[k][:, n:n + 1]),
                         reads=[ores], writes=[tag + "sqj", hk + "ss%d" % n])
                    rstd_ops(ssb[k][:, n:n + 1], 128, hk + "ss%d" % n)
                    P.op("dve", lambda e, k=k, n=n, oc=oc, h=h: e.scalar_tensor_tensor(out=ogf[k], in0=oc, scalar=ssb[k][:, n:n + 1],
                                                                                      in1=gBrow[:, h * 128:(h + 1) * 128], op0=ALU.mult, op1=ALU.mult),
                         reads=[ores, hk + "ss%d" % n, tag + "gB"], writes=[hk + "og"])
                    P.op("pool", lambda e, k=k, par=par, n=n: e.tensor_tensor(out=ybf[k][par], in0=ogf[k], in1=sg[k][:, n, :], op=ALU.mult),
                         reads=[hk + "og", hk + "sg"], writes=[hk + "yb%d" % par])
                    tb2 = 6 + ((n * NH + k + 1) % 2)
                    P.op("pe", lambda e, k=k, par=par, tb2=tb2: e.transpose(out=pbf(tb2, 256, 64), in_=ybf[k][par], identity=ident[0:64, 0:64]),
                         reads=[hk + "yb%d" % par], writes=["pf%d" % tb2])
                    yT_store(4 + h, n * C, C, pbf(tb2, 256, 64), None, ["pf%d" % tb2])
            P.barrier()

    def phase_C(li):
        tag = "C."
        sm = smallf
        scale = 128.0 ** -0.5
        CT = cfg.lim.get("C_tiles", NT)
        for g in range(cfg.lim.get("C_groups", 2)):
            AR.reset()
            qT = [AR.bf([128, T]) for _ in range(3)]
            kcT = AR.bf([128, T])
            vcT = AR.bf([128, T])
            ksT = AR.bf([128, T])
            kwT = AR.bf([128, T])
            vsa = AR.bf([128, NT, VP])
            vwa = AR.bf([128, NT, VP])
            cmask = AR.bf([128, T])
            expand = AR.bf([32, T])
            kcc = AR.bf([128, 128])
            vca = AR.bf([128, 168])
            mark_b = AR.ob
            xs = AR.bf([128, 32, 127])
            w1b = AR.bf([128, 32, 256])
            w2b = AR.bf([128, 2, 128])
            glb = AR.bf([128, 256])
            glT = AR.bf([128, 2, 128])
            ocg = [AR.f32([128, NT, 128]) for _ in range(3)]
            imp = AR.f32([128, NT, 32])
            selb = AR.f32([128, NT, 32])
            glg = AR.f32([128, NT, 9])
            peT = AR.bf([128, 32])
            pe_b = AR.bf([32, 128])
            gx = [AR.f32([128, 256]) for _ in range(3)]
            sc1 = AR.f32([128, 32])
            sc2 = AR.f32([128, 32])
            acc = [AR.f32([128, 128]) for _ in range(2)]
            m8 = sm[:, 0:8]
            m8b = sm[:, 8:16]
            thr = sm[:, 16:32]
            rc = sm[:, 32:80]
            rsw = sm[:, 80:96]
            csw = sm[:, 96:112]
            rcg = sm[:, 112:160]
            for j in range(3):
                P.dma("sp", qT[j], fmb[FMB_BASE["c_q"] + g * 3 + j], reads=["scr.c_q"], writes=[tag + "qT%d" % j], chan=tag + "qT%d" % j)
            for nm, buf in (("c_kc", kcT), ("c_vc", vcT), ("c_ks", ksT), ("c_kw", kwT)):
                P.dma("sp", buf, fmb[FMB_BASE[nm] + g], reads=["scr." + nm], writes=[tag + nm], chan=tag + nm)
            for nm, buf in (("c_vs", vsa), ("c_vw", vwa)):
                P.dma("sp", buf, tmv[TMV_BASE[nm] + g], writes=[tag + nm], chan=tag + nm)
            P.dma("sp", glg, tmg[g], writes=[tag + "glg"], chan=tag + "glg")
            P.op("act", lambda e: e.activation(out=glg, in_=glg, func=AF.Sigmoid), reads=[tag + "glg"], writes=[tag + "glg"])
            P.dma("sp", cmask, c_cmask_d[:, :], writes=[tag + "cmask"], chan=tag + "cmask")
            P.dma("sp", expand, c_expand_d[:, :], writes=[tag + "expand"], chan=tag + "expand")
            P.dma("sp", selb, c_selbias_d[:, :, :], writes=[tag + "selb"], chan=tag + "selb")
            P.op("pool", lambda e: e.memset(vca, 0.0), writes=[tag + "vca"])
            P.op("pool", lambda e: e.memset(vca[:, 128:129], 1.0), reads=[tag + "vca"], writes=[tag + "vca"])
            P.dma("sp", vca[:, 129:161], c_ov_d[:, :], reads=[tag + "vca"], writes=[tag + "vca_ov"], chan=tag + "vca_ov")
            for which, srcT, pen, w1n, w2n in (("k", kcT, "nsa_pe_k", "nsa_ck_w1", "nsa_ck_w2"),
                                               ("v", vcT, "nsa_pe_v", "nsa_cv_w1", "nsa_cv_w2")):
                P.dma("pool", pe_b, W[pen][li], writes=[tag + "pe_b"], chan=tag + "pe_b")
                P.op("pe", lambda e: e.transpose(out=pbf(7, 0, 32), in_=pe_b, identity=ident[0:32, 0:32]), reads=[tag + "pe_b"], writes=["pf7"])
                evac(peT, pbf(7, 0, 32), reads=["pf7"], writes=[tag + "peT"])
                P.dma("pool", w1b, W[w1n][li].rearrange("(l d) m -> d l m", d=128), writes=[tag + "w1b"], chan=tag + "w1b")
                P.dma("pool", w2b, W[w2n][li].rearrange("(c p) d -> p c d", p=128), writes=[tag + "w2b"], chan=tag + "w2b")
                sv = srcT.rearrange("p (n r) -> p n r", r=16)
                srcres = tag + ("c_kc" if which == "k" else "c_vc")
                P.op("dve", lambda e, sv=sv: e.tensor_tensor(
                    out=xs[:, 0:16, :], in0=sv[:, 0:127, :].rearrange("p n l -> p l n"),
                    in1=peT[:, 0:16].unsqueeze(2).to_broadcast([128, 16, 127]), op=ALU.add),
                    reads=[srcres, tag + "peT"], writes=[tag + "xs"])
                P.op("dve", lambda e, sv=sv: e.tensor_tensor(
                    out=xs[:, 16:32, :], in0=sv[:, 1:128, :].rearrange("p n l -> p l n"),
                    in1=peT[:, 16:32].unsqueeze(2).to_broadcast([128, 16, 127]), op=ALU.add),
                    reads=[srcres, tag + "peT", tag + "xs"], writes=[tag + "xs"])
                for l in range(32):
                    P.op("pe", lambda e, l=l: e.matmul(pf[5][0:127, 0:256], lhsT=xs[:, l, :], rhs=w1b[:, l, :],
                                                       start=(l == 0), stop=(l == 31)),
                         reads=[tag + "xs", tag + "w1b"], writes=["pf5"])
                xg = gx[2][0:127, :]
                P.op("act", lambda e: e.copy(out=xg, in_=pf[5][0:127, 0:256]), reads=["pf5"], writes=[tag + "gx2"])
                P.op("dve", lambda e: e.tensor_tensor(out=gx[0][0:127, :], in0=xg, in1=xg, op=ALU.mult),
                     reads=[tag + "gx2"], writes=[tag + "gx0"])
                P.op("dve", lambda e: e.tensor_scalar(out=gx[0][0:127, :], in0=gx[0][0:127, :], scalar1=0.044715, scalar2=1.0,
                                                      op0=ALU.mult, op1=ALU.add), reads=[tag + "gx0"], writes=[tag + "gx0"])
                P.op("dve", lambda e: e.tensor_tensor(out=gx[0][0:127, :], in0=gx[0][0:127, :], in1=xg, op=ALU.mult),
                     reads=[tag + "gx0", tag + "gx2"], writes=[tag + "gx0"])
                P.op("act", lambda e: e.activation(out=gx[1][0:127, :], in_=gx[0][0:127, :], func=AF.Sigmoid, scale=1.5957691216057308),
                     reads=[tag + "gx0"], writes=[tag + "gx1"])
                P.op("dve", lambda e: e.tensor_tensor(out=glb[0:127, :], in0=gx[1][0:127, :], in1=xg, op=ALU.mult),
                     reads=[tag + "gx1", tag + "gx2"], writes=[tag + "glb"])
                for c in range(2):
                    P.op("pe", lambda e, c=c: e.transpose(out=pbf(6, c * 128, 127), in_=glb[0:127, c * 128:(c + 1) * 128],
                                                          identity=ident[0:127, 0:127]),
                         reads=[tag + "glb", "cb"], writes=["pf6"])
                evac(glT[:, :, 0:127], pbf(6, 0, 256).rearrange("p (c n) -> p c n", c=2)[:, :, 0:127], reads=["pf6"], writes=[tag + "glT"])
                if which == "k":
                    for c in range(2):
                        P.op("pe", lambda e, c=c: e.matmul(pf[5][:, 256:383], lhsT=w2b[:, c, :], rhs=glT[:, c, 0:127],
                                                           start=(c == 0), stop=(c == 1)),
                             reads=[tag + "w2b", tag + "glT"], writes=["pf5"])
                    evac(kcc[:, 0:127], pf[5][:, 256:383], reads=["pf5"], writes=[tag + "kcc"])
                else:
                    for c in range(2):
                        P.op("pe", lambda e, c=c: e.matmul(pf[5][0:127, 256:384], lhsT=glT[:, c, 0:127], rhs=w2b[:, c, :],
                                                           start=(c == 0), stop=(c == 1)),
                             reads=[tag + "w2b", tag + "glT"], writes=["pf5"])
                    evac(vca[0:127, 0:128], pf[5][0:127, 256:384], reads=["pf5", tag + "vca"], writes=[tag + "vca_v"])
            P.barrier()
            AR.ob = mark_b
            PTc = [AR.bf([128, 512]) for _ in range(2)]
            PT = [AR.bf([128, 512]) for _ in range(4)]
            nsel = AR.bf([128, 32])
            nselT = AR.bf([32, NT, 128])
            accb = [AR.bf([128, 128]) for _ in range(2)]
            cnt = 0
            for j in range(3):
                for qb in range((CT + 3) // 4):
                    sb = cnt % 2
                    pc = cnt % 2
                    cnt += 1
                    P.op("pe", lambda e, j=j, qb=qb, sb=sb: e.matmul(pf[sb][0:127, :], lhsT=kcc[:, 0:127], rhs=qT[j][:, qb * 512:(qb + 1) * 512],
                                                                    start=True, stop=True),
                         reads=[tag + "kcc", tag + "qT%d" % j], writes=["pf%d" % sb])
                    P.op("act", lambda e, sb=sb, pc=pc: e.activation(out=PTc[pc][0:127, :], in_=pf[sb][0:127, :], func=AF.Exp, scale=scale),
                         reads=["pf%d" % sb], writes=[tag + "PTc%d" % pc])
                    P.op("pool", lambda e, pc=pc, qb=qb: e.tensor_tensor(out=PTc[pc][0:127, :], in0=PTc[pc][0:127, :],
                                                                         in1=cmask[0:127, qb * 512:(qb + 1) * 512], op=ALU.mult),
                         reads=[tag + "PTc%d" % pc, tag + "cmask"], writes=[tag + "PTc%d" % pc])
                    for it4 in range(4):
                        it = qb * 4 + it4
                        half = it % 2
                        oc = pf[4 + half][:, 0:161]
                        ores = "pf%d" % (4 + half)
                        P.op("pe", lambda e, pc=pc, it4=it4, oc=oc: e.matmul(oc, lhsT=PTc[pc][0:127, it4 * 128:(it4 + 1) * 128],
                                                                             rhs=vca[0:127, 0:161], start=True, stop=True),
                             reads=[tag + "PTc%d" % pc, tag + "vca", tag + "vca_ov", tag + "vca_v"], writes=[ores])
                        u = j * NT + it
                        P.op("dve", lambda e, oc=oc, u=u: e.tensor_scalar(out=rc[:, u:u + 1], in0=oc[:, 128:129], scalar1=1e-30, scalar2=None,
                                                                          op0=ALU.max), reads=[ores], writes=[tag + "rc%d" % u])
                        P.op("dve", lambda e, u=u: e.reciprocal(out=rc[:, u:u + 1], in_=rc[:, u:u + 1]),
                             reads=[tag + "rc%d" % u], writes=[tag + "rc%d" % u])
                        if j == 0:
                            P.op("dve", lambda e, oc=oc, u=u, it=it: e.tensor_scalar(out=imp[:, it, :], in0=oc[:, 129:161], scalar1=rc[:, u:u + 1],
                                                                                     scalar2=None, op0=ALU.mult),
                                 reads=[ores, tag + "rc%d" % u], writes=[tag + "imp%d" % it])
                        else:
                            P.op("dve", lambda e, oc=oc, u=u, it=it: e.scalar_tensor_tensor(out=imp[:, it, :], in0=oc[:, 129:161], scalar=rc[:, u:u + 1],
                                                                                            in1=imp[:, it, :], op0=ALU.mult, op1=ALU.add),
                                 reads=[ores, tag + "rc%d" % u, tag + "imp%d" % it], writes=[tag + "imp%d" % it])
                        P.op("dve", lambda e, u=u, it=it, j=j: e.tensor_tensor(out=rcg[:, u:u + 1], in0=rc[:, u:u + 1], in1=glg[:, it, 3 * j:3 * j + 1], op=ALU.mult),
                             reads=[tag + "rc%d" % u, tag + "glg"], writes=[tag + "rcg%d" % u])
                        if "c" not in cfg.lim.get("C_terms", "csw"):
                            P.op("dve", lambda e, u=u: e.memset(rcg[:, u:u + 1], 0.0), reads=[tag + "rcg%d" % u], writes=[tag + "rcg%d" % u])
                        P.op("act", lambda e, oc=oc, u=u, it=it, j=j: e.activation(out=ocg[j][:, it, :], in_=oc[:, 0:128], func=AF.Copy, scale=rcg[:, u:u + 1]),
                             reads=[ores, tag + "rcg%d" % u], writes=[tag + "ocg%d.%d" % (j, it)])
            for it in range(CT):
                P.op("dve", lambda e, it=it: e.tensor_tensor(out=sc1, in0=imp[:, it, :], in1=selb[:, it, :], op=ALU.add),
                     reads=[tag + "imp%d" % it, tag + "selb"], writes=[tag + "sc1"])
                P.op("dve", lambda e: e.max(out=m8, in_=sc1), reads=[tag + "sc1"], writes=[tag + "m8"])
                P.op("dve", lambda e: e.match_replace(out=sc2, in_to_replace=m8, in_values=sc1, imm_value=-1.0e30),
                     reads=[tag + "sc1", tag + "m8"], writes=[tag + "sc2"])
                P.op("dve", lambda e: e.max(out=m8b, in_=sc2), reads=[tag + "sc2"], writes=[tag + "m8b"])
                P.op("dve", lambda e, it=it: e.tensor_reduce(out=thr[:, it:it + 1], in_=m8b, axis=AX.X, op=ALU.min),
                     reads=[tag + "m8b"], writes=[tag + "thr%d" % it])
                P.op("dve", lambda e, it=it: e.tensor_scalar(out=nsel, in0=sc1, scalar1=thr[:, it:it + 1], scalar2=None, op0=ALU.is_lt),
                     reads=[tag + "sc1", tag + "thr%d" % it], writes=[tag + "nsel"])
                P.op("pe", lambda e: e.transpose(out=pbf(6, 512, 128)[0:32, :], in_=nsel, identity=ident),
                     reads=[tag + "nsel", "cb"], writes=["pf6"])
                evac(nselT[:, it, :], pbf(6, 512, 128)[0:32, :], reads=["pf6"], writes=[tag + "nselT%d" % it])
            grp = 0
            for j in range(3):
                for i in range(CT):
                    ob = 2 + (i % 2)
                    o_s = pf[ob][:, 0:129]
                    o_w = pf[ob][:, 256:385]
                    ores = "pf%d" % ob
                    for branch in ("s", "w"):
                        kts_all = list(range(0, i + 1)) if branch == "s" else list(range(max(0, i - 4), i + 1))
                        kTb = ksT if branch == "s" else kwT
                        kres = tag + ("c_ks" if branch == "s" else "c_kw")
                        vab = vsa if branch == "s" else vwa
                        vres = [tag + ("c_vs" if branch == "s" else "c_vw"), tag + ("c_vs1" if branch == "s" else "c_vw1")]
                        oc = o_s if branch == "s" else o_w
                        for g0 in range(0, len(kts_all), 4):
                            kts = kts_all[g0:g0 + 4]
                            sb = grp % 2
                            ps = grp % 4
                            grp += 1
                            for jj, kt in enumerate(kts):
                                extra = []
                                if branch == "s":
                                    extra.append(("sel", kt))
                                if kt == i:
                                    extra.append(("negc", kt))
                                if branch == "w" and kt == i - 4:
                                    extra.append(("negw", kt))
                                dst = pf[sb][:, jj * 128:(jj + 1) * 128]
                                P.op("pe", lambda e, kTb=kTb, kt=kt, j=j, i=i, dst=dst, extra=extra: e.matmul(
                                    dst, lhsT=kTb[:, kt * 128:(kt + 1) * 128], rhs=qT[j][:, i * 128:(i + 1) * 128],
                                    start=True, stop=(len(extra) == 0)),
                                    reads=[kres, tag + "qT%d" % j], writes=["pf%d" % sb])
                                for xi, (kind, _) in enumerate(extra):
                                    last = (xi == len(extra) - 1)
                                    if kind == "sel":
                                        P.op("pe", lambda e, kt=kt, i=i, dst=dst, last=last: e.matmul(
                                            dst, lhsT=expand[0:32, kt * 128:(kt + 1) * 128], rhs=nselT[0:32, i, :], start=False, stop=last),
                                            reads=[tag + "expand", tag + "nselT%d" % i], writes=["pf%d" % sb])
                                    else:
                                        mk = negc if kind == "negc" else negw
                                        P.op("pe", lambda e, dst=dst, last=last, mk=mk: e.matmul(dst, lhsT=ident, rhs=mk, start=False, stop=last),
                                             reads=["cb"], writes=["pf%d" % sb])
                            n = len(kts) * 128
                            P.op("act", lambda e, sb=sb, ps=ps, n=n: e.activation(out=PT[ps][:, 0:n], in_=pf[sb][:, 0:n], func=AF.Exp, scale=scale),
                                 reads=["pf%d" % sb], writes=[tag + "PT%d" % ps])
                            for jj, kt in enumerate(kts):
                                P.op("pe", lambda e, ps=ps, jj=jj, kt=kt, oc=oc, vab=vab, first=(kt == kts_all[0]), lastk=(kt == i): e.matmul(
                                    oc, lhsT=PT[ps][:, jj * 128:(jj + 1) * 128], rhs=vab[:, kt, 0:129], start=first, stop=lastk),
                                    reads=[tag + "PT%d" % ps] + vres, writes=[ores])
                    s = (j * NT + i) % 2
                    u = j * NT + i
                    rs = rsw[:, 2 * s:2 * s + 2]
                    cs_ = csw[:, 2 * s:2 * s + 2]
                    P.op("dve", lambda e, ob=ob, rs=rs, i=i: e.tensor_tensor(out=rs, in0=pf[ob][:, 128:512:256], in1=phz[:, i, :], op=ALU.add),
                         reads=[ores], writes=[tag + "rs%d" % s])
                    P.op("dve", lambda e, rs=rs: e.reciprocal(out=rs, in_=rs),
                         reads=[tag + "rs%d" % s], writes=[tag + "rs%d" % s])
                    P.op("dve", lambda e, rs=rs, cs_=cs_, i=i, j=j: e.tensor_tensor(out=cs_, in0=rs, in1=glg[:, i, 3 * j + 1:3 * j + 3], op=ALU.mult),
                         reads=[tag + "rs%d" % s, tag + "glg"], writes=[tag + "cs%d" % s])
                    if "s" not in cfg.lim.get("C_terms", "csw"):
                        P.op("dve", lambda e, cs_=cs_: e.memset(cs_[:, 0:1], 0.0), reads=[tag + "cs%d" % s], writes=[tag + "cs%d" % s])
                    if "w" not in cfg.lim.get("C_terms", "csw"):
                        P.op("dve", lambda e, cs_=cs_: e.memset(cs_[:, 1:2], 0.0), reads=[tag + "cs%d" % s], writes=[tag + "cs%d" % s])
                    P.op("dve", lambda e, s=s, cs_=cs_, j=j, i=i, o_s=o_s: e.scalar_tensor_tensor(
                        out=acc[s], in0=o_s[:, 0:128], scalar=cs_[:, 0:1], in1=ocg[j][:, i, :], op0=ALU.mult, op1=ALU.add),
                        reads=[ores, tag + "cs%d" % s, tag + "ocg%d.%d" % (j, i)], writes=[tag + "acc%d" % s])
                    P.op("dve", lambda e, s=s, cs_=cs_, o_w=o_w: e.scalar_tensor_tensor(
                        out=accb[s], in0=o_w[:, 0:128], scalar=cs_[:, 1:2], in1=acc[s], op0=ALU.mult, op1=ALU.add),
                        reads=[ores, tag + "cs%d" % s, tag + "acc%d" % s], writes=[tag + "accb%d" % s])
                    tb = 6 + (u % 2)
                    P.op("pe", lambda e, s=s, tb=tb: e.transpose(out=pbf(tb, 0, 128), in_=accb[s], identity=ident),
                         reads=[tag + "accb%d" % s, "cb"], writes=["pf%d" % tb])
                    yT_store(10 + g * 3 + j, i * 128, 128, pbf(tb, 0, 128), None, ["pf%d" % tb])
            P.barrier()

    def phase_post(li, xsrc, xdst):
        tag = "post."
        AR.reset()
        wb = [AR.bf([128, 16, 512]) for _ in range(2)]
        xs_ = [AR.f32([128, 512]) for _ in range(3)]
        wv = W["w_out"][li].rearrange("(c p) n -> p c n", p=128)
        k = 0
        for pn in range(4):
            s = pn % 2
            wres = tag + "wo%d" % s
            P.dma("pool", wb[s], wv[:, :, pn * 512:(pn + 1) * 512], writes=[wres], chan=wres)
            for it in range(NT):
                bank = k % 4
                xs = k % 3
                k += 1
                xres = tag + "xs%d" % xs
                P.dma("sp", xs_[xs], xsrc[it * 128:(it + 1) * 128, pn * 512:(pn + 1) * 512], writes=[xres], chan=xres)
                for c in range(16):
                    P.op("pe", lambda e, s=s, c=c, it=it, bank=bank: e.matmul(
                        pf[bank][:, :], lhsT=hT3[:, c, it * 128:(it + 1) * 128], rhs=wb[s][:, c, :], start=(c == 0), stop=(c == 15)),
                        reads=[wres] + ["yT.%d" % cc for cc in ([c] if True else [])], writes=["pf%d" % bank])
                P.op("dve", lambda e, xs=xs, bank=bank: e.tensor_tensor(out=xs_[xs], in0=xs_[xs], in1=pf[bank][:, :], op=ALU.add),
                     reads=["pf%d" % bank, xres], writes=[xres])
                P.dma("sp", xmid[it * 128:(it + 1) * 128, pn * 512:(pn + 1) * 512], xs_[xs], reads=[xres], chan=xres)
        P.barrier()
        AR.reset()
        TB = 512
        NTB = TB // 128
        hT2 = actT[:, 0:16 * TB].rearrange("p (c t) -> p c t", c=16)
        uT = actT[:, 16 * TB:16 * TB + 44 * TB].rearrange("p (c t) -> p c t", c=44)
        xt = [AR.f32([128, D]) for _ in range(2)]
        gbc = AR.f32([128, D])
        xo = [AR.f32([128, 512]) for _ in range(3)]
        hn = [AR.bf([128, D]) for _ in range(2)]
        wg = [AR.bf([128, 16, 256]) for _ in range(2)]
        wu = [AR.bf([128, 16, 256]) for _ in range(2)]
        wd = [AR.bf([128, 11, 512]) for _ in range(3)]
        sgt = [AR.bf([128, 512]) for _ in range(2)]
        ssb = smallf[:, 0:16]
        P.dma("sp", gbc, W["ffn_norm"][li].partition_broadcast(128), writes=[tag + "gbc"], chan=tag + "gbc")
        wgv = W["w_gate"][li].rearrange("(c p) n -> p c n", p=128)
        wuv = W["w_up"][li].rearrange("(c p) n -> p c n", p=128)
        wdv = W["w_down"][li].rearrange("(c p) n -> p c n", p=128)
        kk = 0
        kd_ = 0
        for blk in range(T // TB):
            t0 = blk * TB
            P.op("dve", lambda e: e.memset(ssb, 0.0), writes=[tag + "ss%d" % i for i in range(NTB)])
            norm_to_T(tag, xmid[t0:t0 + TB, :], None, hT2, 0, NTB, gbc, xt, hn, ssb)
            for pn in range(DFF // 256):
                s = pn % 2
                gres, ures = tag + "wg%d" % s, tag + "wu%d" % s
                P.dma("pool", wg[s], wgv[:, :, pn * 256:(pn + 1) * 256], writes=[gres], chan=gres)
                P.dma("pool", wu[s], wuv[:, :, pn * 256:(pn + 1) * 256], writes=[ures], chan=ures)
                for jt in range(2):
                    ft = pn * 2 + jt
                    bg = (kk % 2) * 2
                    bu = bg + 1
                    sgi = kk % 2
                    kk += 1
                    for c in range(16):
                        P.op("pe", lambda e, s=s, c=c, jt=jt, bg=bg: e.matmul(pf[bg][:, :], lhsT=wg[s][:, c, jt * 128:(jt + 1) * 128],
                                                                              rhs=hT2[:, c, :], start=(c == 0), stop=(c == 15)),
                             reads=[gres, tag + "dstT"], writes=["pf%d" % bg])
                    for c in range(16):
                        P.op("pe", lambda e, s=s, c=c, jt=jt, bu=bu: e.matmul(pf[bu][:, :], lhsT=wu[s][:, c, jt * 128:(jt + 1) * 128],
                                                                              rhs=hT2[:, c, :], start=(c == 0), stop=(c == 15)),
                             reads=[ures, tag + "dstT"], writes=["pf%d" % bu])
                    P.op("act", lambda e, bg=bg, sgi=sgi: e.activation(out=sgt[sgi], in_=pf[bg][:, :], func=AF.Silu),
                         reads=["pf%d" % bg], writes=[tag + "sg%d" % sgi])
                    P.op("dve", lambda e, bu=bu, sgi=sgi, ft=ft: e.tensor_tensor(out=uT[:, ft, :], in0=sgt[sgi], in1=pf[bu][:, :], op=ALU.mult),
                         reads=["pf%d" % bu, tag + "sg%d" % sgi], writes=[tag + "uT"])
            for pn in range(4):
                for half in range(4):
                    s = kd_ % 3
                    kd_ += 1
                    dres = tag + "wd%d" % s
                    P.dma("pool", wd[s], wdv[:, half * 11:(half + 1) * 11, pn * 512:(pn + 1) * 512], writes=[dres], chan=dres)
                    for it in range(NTB):
                        bank = 4 + it
                        for c in range(11):
                            cc = half * 11 + c
                            P.op("pe", lambda e, s=s, c=c, cc=cc, it=it, bank=bank: e.matmul(
                                pf[bank][:, :], lhsT=uT[:, cc, it * 128:(it + 1) * 128], rhs=wd[s][:, c, :],
                                start=(cc == 0), stop=(cc == 43)),
                                reads=[dres, tag + "uT"], writes=["pf%d" % bank])
                for it in range(NTB):
                    bank = 4 + it
                    xi = (pn * NTB + it) % 3
                    xres = tag + "xo%d" % xi
                    r0 = t0 + it * 128
                    P.dma("sp", xo[xi], xmid[r0:r0 + 128, pn * 512:(pn + 1) * 512], writes=[xres], chan=xres)
                    P.op("dve", lambda e, xi=xi, bank=bank: e.tensor_tensor(out=xo[xi], in0=xo[xi], in1=pf[bank][:, :], op=ALU.add),
                         reads=["pf%d" % bank, xres], writes=[xres])
                    P.dma("sp", xdst[r0:r0 + 128, pn * 512:(pn + 1) * 512], xo[xi], reads=[xres], chan=xres)
        P.barrier()

    def phase_final(xsrc):
        tag = "fin."
        AR.reset()
        xt = [AR.f32([128, D]) for _ in range(2)]
        gbc = AR.f32([128, D])
        yo = [AR.f32([128, D]) for _ in range(2)]
        sq = AR.bf([128, D])
        ssb = smallf[:, 0:16]
        P.op("dve", lambda e: e.memset(ssb, 0.0), writes=[tag + "ss%d" % i for i in range(16)])
        P.dma("sp", gbc, final_norm.partition_broadcast(128), writes=[tag + "gbc"], chan=tag + "gbc")
        for i in range(NT):
            s = i % 2
            P.dma("sp", xt[s], xsrc[i * 128:(i + 1) * 128, :], writes=[tag + "xt%d" % s], chan=tag + "xt%d" % s)
            P.op("act", lambda e, s=s, i=i: e.activation(out=sq, in_=xt[s], func=AF.Square, accum_out=ssb[:, i:i + 1]),
                 reads=[tag + "xt%d" % s], writes=[tag + "sq", tag + "ss%d" % i])
            rstd_ops(ssb[:, i:i + 1], D, tag + "ss%d" % i)
            P.op("dve", lambda e, s=s, i=i: e.scalar_tensor_tensor(out=yo[s], in0=xt[s], scalar=ssb[:, i:i + 1], in1=gbc,
                                                                   op0=ALU.mult, op1=ALU.mult),
                 reads=[tag + "xt%d" % s, tag + "ss%d" % i, tag + "gbc"], writes=[tag + "yo%d" % s])
            P.dma("sp", out[i * 128:(i + 1) * 128, :], yo[s], reads=[tag + "yo%d" % s], writes=["out.%d" % i], chan=tag + "yo%d" % s)
        P.op("sp", None, reads=["out.%d" % i for i in range(NT)])

    if cfg.lim or yT_dbg is not None:
        P.op("pool", lambda e: e.memset(actT[:, :], 0.0), writes=["actT0"])
    P.barrier()
    xcur = x_in
    for li in range(NL):
        last = (li == NL - 1)
        if "pre" in cfg.phases:
            phase_pre(li, xcur)
        if "yT_in" in cfg.taps:
            P.dma("sp", hT3, yT_in[:, :, :], writes=["yTall"], chan="yTin")
            P.barrier()
        if "A" in cfg.phases:
            phase_A(li)
        if "B" in cfg.phases:
            phase_B(li)
        if "C" in cfg.phases:
            phase_C(li)
        if yT_dbg is not None:
            P.dma("sp", yT_dbg[:, :, :], hT3, writes=["yTdbg"], chan="yTdbg")
            P.barrier()
        if "post" in cfg.phases:
            xdst = out if (last and not cfg.final) else xbuf[li % 2]
            phase_post(li, xcur, xdst)
            xcur = xdst
    if cfg.final:
        phase_final(xcur)
    P.barrier()
    P.emit()
    return nc, P


_CACHE = {}


def _get_prog(key, cfg):
    if key not in _CACHE:
        _CACHE[key] = build(cfg)[0]
    return _CACHE[key]


FUSED = True


def kernel(**inputs):
    inputs = {k: np.asarray(v) for k, v in inputs.items()}
    B = inputs["x"].shape[0]
    hc = host_consts()
    x = np.ascontiguousarray(inputs["x"], dtype=np.float32)
    if FUSED:
        nc = _get_prog(("fused",), Cfg(nl=DEPTH, final=True))
        lc = layer_consts(list(range(DEPTH)))
        in_maps = []
        for b in range(B):
            m = {"x": x[b], "hg_gamma": inputs["hg_gamma"], "final_norm": inputs["final_norm"], "lconst": lc}
            for n, _ in WSHAPES:
                m[n] = np.ascontiguousarray(inputs[n], dtype=np.float32)
            m.update(hc)
            in_maps.append(m)
        res = run_bass_kernel_spmd(nc, in_maps, core_ids=list(range(B)))
        return np.stack([np.asarray(res.results[b]["out"]) for b in range(B)], axis=0).astype(np.float32)
    cur = [x[b] for b in range(B)]
    for l in range(DEPTH):
        final = (l == DEPTH - 1)
        nc = _get_prog(("layer", final), Cfg(nl=1, final=final))
        lc = layer_consts([l])
        in_maps = []
        for b in range(B):
            m = {"x": np.ascontiguousarray(cur[b]), "hg_gamma": inputs["hg_gamma"], "final_norm": inputs["final_norm"],
                 "lconst": lc}
            for n, _ in WSHAPES:
                m[n] = np.ascontiguousarray(inputs[n][l:l + 1])
            m.update(hc)
            in_maps.append(m)
        res = run_bass_kernel_spmd(nc, in_maps, core_ids=list(range(B)))
        cur = [np.asarray(res.results[b]["out"]) for b in range(B)]
    return np.stack(cur, axis=0).astype(np.float32)
```

```python
import contextlib
import math
import numpy as np
import ml_dtypes
import concourse.bass as bass
import concourse.mybir as mybir
from concourse.bass_utils import run_bass_kernel_spmd

F32 = mybir.dt.float32
BF16 = mybir.dt.bfloat16
AF = mybir.ActivationFunctionType
ALU = mybir.AluOpType
AX = mybir.AxisListType

T = 2048
D = 2048
DIN = 6930
DFF = 5632
DEPTH = 4
NT = T // 128
EPS = 1e-6
NEG = -30000.0

SEG = [
    ("a_q", 0, 512, "FM", "b"), ("a_k", 512, 512, "FM", "b"), ("a_v", 1024, 512, "TM", "b"),
    ("b_f", 1536, 768, "FM", "f"), ("b_q", 2304, 768, "FM", "f"), ("b_i", 3072, 768, "TM", "b"),
    ("b_g", 3840, 768, "TM", "f"), ("c_q", 4608, 768, "FM", "b"), ("c_kc", 5376, 256, "FM", "b"),
    ("c_vc", 5632, 256, "FM", "b"), ("c_ks", 5888, 256, "FM", "b"), ("c_vs", 6144, 256, "TM", "b"),
    ("c_kw", 6400, 256, "FM", "b"), ("c_vw", 6656, 256, "TM", "b"), ("c_g", 6912, 18, "TM", "f"),
]
FMB_BASE = {"a_q": 0, "a_k": 4, "c_q": 8, "c_kc": 14, "c_vc": 16, "c_ks": 18, "c_kw": 20}
NFMB = 22
FMF_BASE = {"b_f": 0, "b_q": 6}
NFMF = 12
TMV_BASE = {"a_v": 0, "c_vs": 4, "c_vw": 6}
NTMV = 8
VP = 132

WSHAPES = [
    ("attn_norm", [D]), ("w_in", [D, DIN]), ("da_lam_q1", [64]), ("da_lam_k1", [64]),
    ("da_lam_q2", [64]), ("da_lam_k2", [64]), ("da_norm", [4, 128]), ("hg_norm", [6, 128]),
    ("nsa_pe_k", [32, 128]), ("nsa_pe_v", [32, 128]), ("nsa_ck_w1", [4096, 256]),
    ("nsa_ck_w2", [256, 128]), ("nsa_cv_w1", [4096, 256]), ("nsa_cv_w2", [256, 128]),
    ("w_out", [D, D]), ("ffn_norm", [D]), ("w_gate", [D, DFF]), ("w_up", [D, DFF]),
    ("w_down", [DFF, D]),
]


class _Op:
    __slots__ = ("eng", "fn", "deps", "sig", "sem", "val", "chan")


class Prog:
    ENGS = ("pe", "act", "dve", "pool", "sp")

    def __init__(self, nc):
        self.nc = nc
        self.ops = {e: [] for e in self.ENGS}
        self.res = {}
        self.stack = contextlib.ExitStack()
        self.last = {e: None for e in self.ENGS}
        self.pending_dma = []

    def sbuf(self, name, shape, dt):
        return self.stack.enter_context(self.nc.sbuf_tensor(name, list(shape), dt))

    def psum(self, name, shape, dt):
        return self.stack.enter_context(self.nc.psum_tensor(name, list(shape), dt))

    def op(self, eng, fn, reads=(), writes=(), chan=None):
        o = _Op()
        o.eng = eng
        o.fn = fn
        o.chan = chan
        o.sig = chan is not None
        o.sem = None
        o.val = 0
        deps = []
        for r in reads:
            st = self.res.get(r)
            if st is not None and st[0] is not None:
                deps.append(st[0])
        for w in writes:
            st = self.res.get(w)
            if st is not None:
                if st[0] is not None:
                    deps.append(st[0])
                deps.extend(st[1])
        seen = set()
        dd = []
        for d in deps:
            if id(d) in seen or d is o:
                continue
            seen.add(id(d))
            dd.append(d)
        o.deps = dd
        self.ops[eng].append(o)
        for r in reads:
            st = self.res.get(r)
            if st is None:
                self.res[r] = [None, [o]]
            else:
                st[1].append(o)
        for w in writes:
            self.res[w] = [o, []]
        if fn is not None:
            self.last[eng] = o
        if chan is not None:
            self.pending_dma.append(o)
        return o

    def dma(self, eng, out, in_, reads=(), writes=(), chan=None, **kw):
        assert chan is not None
        return self.op(eng, lambda e: e.dma_start(out=out, in_=in_, **kw),
                       reads=reads, writes=writes, chan=chan)

    def barrier(self):
        lasts = [o for o in self.last.values() if o is not None] + list(self.pending_dma)
        self.pending_dma = []
        for e in self.ENGS:
            o = self.op(e, None)
            o.deps = list(lasts)
        self.res = {}

    def emit(self):
        nc = self.nc
        for e in self.ENGS:
            for o in self.ops[e]:
                for d in o.deps:
                    if d.chan is None:
                        if d.eng == "pe" and o.eng == "pe":
                            continue
                        d.sig = True
        sems = {}
        for e in self.ENGS:
            sems[e] = self.stack.enter_context(nc.semaphore("sem_" + e))
        chans = sorted({o.chan for e in self.ENGS for o in self.ops[e] if o.chan is not None})
        for c in chans:
            sems["c:" + c] = self.stack.enter_context(nc.semaphore("semc_" + c.replace(".", "_")))
        cnt = {}
        for e in self.ENGS:
            n = 0
            for o in self.ops[e]:
                if o.chan is not None:
                    k = "c:" + o.chan
                    cnt[k] = cnt.get(k, 0) + 16
                    o.sem = sems[k]
                    o.val = cnt[k]
                elif o.sig:
                    assert o.fn is not None
                    n += 1
                    o.sem = sems[e]
                    o.val = n
        stats = {}

        def run(ename, eng):
            waited = {}
            nw = 0
            for o in self.ops[ename]:
                need = {}
                for d in o.deps:
                    if d.sem is None:
                        continue
                    if d.chan is None and d.eng == ename and ename == "pe":
                        continue
                    key = id(d.sem)
                    if waited.get(key, 0) >= d.val:
                        continue
                    if key not in need or need[key][1] < d.val:
                        need[key] = (d.sem, d.val)
                for key, (sem, val) in need.items():
                    eng.wait_ge(sem, val)
                    waited[key] = val
                    nw += 1
                if o.fn is None:
                    continue
                inst = o.fn(eng)
                if o.sem is not None:
                    inst.then_inc(o.sem, 16 if o.chan is not None else 1)
            stats[ename] = (len(self.ops[ename]), nw)

        with nc.Block() as block:
            @block.sync
            def _(e):
                run("sp", e)

            @block.tensor
            def _(e):
                run("pe", e)

            @block.scalar
            def _(e):
                run("act", e)

            @block.vector
            def _(e):
                run("dve", e)

            @block.gpsimd
            def _(e):
                run("pool", e)
        self.stats = stats
        self.stack.close()


class Arena:
    def __init__(self, tf, tb, nf, nb):
        self.tf, self.tb, self.nf, self.nb = tf, tb, nf, nb
        self.of = self.ob = 0

    def reset(self):
        self.of = self.ob = 0

    @staticmethod
    def _shape(ap, shape):
        if len(shape) == 2:
            return ap
        if len(shape) == 3:
            return ap.rearrange("p (a b) -> p a b", a=shape[1])
        raise ValueError

    def f32(self, shape):
        n = int(np.prod(shape[1:]))
        n4 = (n + 3) // 4 * 4
        assert self.of + n4 <= self.nf, ("f32 arena overflow", self.of, n4, self.nf)
        ap = self.tf[0:shape[0], self.of:self.of + n]
        self.of += n4
        return self._shape(ap, shape)

    def bf(self, shape):
        n = int(np.prod(shape[1:]))
        n8 = (n + 7) // 8 * 8
        assert self.ob + n8 <= self.nb, ("bf16 arena overflow", self.ob, n8, self.nb)
        ap = self.tb[0:shape[0], self.ob:self.ob + n]
        self.ob += n8
        return self._shape(ap, shape)


def host_consts():
    bf = ml_dtypes.bfloat16
    p = np.arange(128)[:, None]
    j = np.arange(128)[None, :]
    ident = (p == j).astype(np.float32)
    tri = (j >= p).astype(np.float32)
    negc = np.where(j < p, NEG, 0.0).astype(np.float32)
    negw = np.where(p <= j, NEG, 0.0).astype(np.float32)
    cb = np.stack([ident, tri, negc, negw], axis=1).astype(bf)
    n = np.arange(127)[:, None]
    t = np.arange(T)[None, :]
    cmask = (16 * n + 31 <= t).astype(np.float32)
    cmask = np.concatenate([cmask, np.zeros((1, T), np.float32)], 0).astype(bf)
    c_start = np.arange(127) * 16
    s_start = np.arange(32) * 64
    ov = ((c_start[:, None] < s_start[None, :] + 64) & (c_start[:, None] + 32 > s_start[None, :])).astype(np.float32)
    ov = np.concatenate([ov, np.zeros((1, 32), np.float32)], 0).astype(bf)
    tt = np.arange(T)[:, None]
    blk = np.arange(32)[None, :]
    cur = tt // 64
    forced = (blk == 0) | (blk == cur) | (blk == cur - 1)
    valid = blk * 64 <= tt
    sb = np.where(valid, np.where(forced, 1.0e4, 0.0), -1.0e30).astype(np.float32)
    selbias = np.ascontiguousarray(sb.reshape(NT, 128, 32).transpose(1, 0, 2))
    key = np.arange(T)[None, :]
    expand = np.where(np.arange(32)[:, None] == key // 64, NEG, 0.0).astype(bf)
    tq = np.arange(T).reshape(NT, 128).T
    phz = np.zeros((128, NT, 2), np.float32)
    phz[:, :, 1] = np.maximum(0, 511 - tq)
    return {"c_b": cb, "c_cmask": cmask, "c_ov": ov, "c_selbias": selbias, "c_expand": expand, "c_phz": phz,
            "c_identf": np.eye(32, dtype=np.float32)}


def layer_consts(layers):
    lc = np.zeros((len(layers), 128, 8), np.float32)
    for i, l in enumerate(layers):
        lam_init = 0.8 - 0.6 * math.exp(-0.3 * l)
        lc[i, :, 0] = lam_init
        lc[i, :, 1] = 1.0 - lam_init
        for m in range(4):
            lc[i, :, 4 + m] = 1.0 if (1 <= m <= l) else 0.0
    return lc


class Cfg:
    def __init__(self, nl=1, final=False, phases=("pre", "A", "B", "C", "post"), scratch_in=False,
                 taps=(), lim=None):
        self.lim = lim or {}
        self.nl = nl
        self.final = final
        self.phases = phases
        self.scratch_in = scratch_in
        self.taps = taps


def build(cfg):
    nc = bass.Bass("TRN2", target_bir_lowering=False)
    P = Prog(nc)
    NL = cfg.nl

    def dram(name, shape, dt, kind="Internal"):
        return nc.dram_tensor(name, list(shape), dt, kind=kind).ap()

    x_in = dram("x", [T, D], F32, "ExternalInput")
    out = dram("out", [T, D], F32, "ExternalOutput")
    W = {n: dram(n, [NL] + s, F32, "ExternalInput") for n, s in WSHAPES}
    hg_gamma = dram("hg_gamma", [4, 768], F32, "ExternalInput")
    final_norm = dram("final_norm", [D], F32, "ExternalInput")
    lconst_d = dram("lconst", [NL, 128, 8], F32, "ExternalInput")
    c_b_d = dram("c_b", [128, 4, 128], BF16, "ExternalInput")
    c_cmask_d = dram("c_cmask", [128, T], BF16, "ExternalInput")
    c_ov_d = dram("c_ov", [128, 32], BF16, "ExternalInput")
    c_selbias_d = dram("c_selbias", [128, NT, 32], F32, "ExternalInput")
    c_expand_d = dram("c_expand", [32, T], BF16, "ExternalInput")
    c_phz_d = dram("c_phz", [128, NT, 2], F32, "ExternalInput")
    c_identf_d = dram("c_identf", [32, 32], F32, "ExternalInput")

    sk = "ExternalInput" if cfg.scratch_in else ("ExternalOutput" if "scratch" in cfg.taps else "Internal")
    fmb = dram("fmb", [NFMB, 128, T], BF16, sk)
    fmf = dram("fmf", [NFMF, 128, T], F32, sk)
    tmv = dram("tmv", [NTMV, 128, NT, VP], BF16, sk)
    tmbi = dram("tmbi", [6, 64, 32, 128], BF16, sk)
    tmbg = dram("tmbg", [6, 64, 32, 128], F32, sk)
    tmg = dram("tmg", [2, 128, NT, 9], F32, sk)
    xmid = dram("xmid", [T, D], F32, "ExternalOutput" if "xmid" in cfg.taps else "Internal")
    xbuf = [dram("xbuf%d" % i, [T, D], F32) for i in range(2)]
    yT_dbg = dram("yT_dbg", [128, 16, T], BF16, "ExternalOutput") if "yT" in cfg.taps else None
    yT_in = dram("yT_in", [128, 16, T], BF16, "ExternalInput") if "yT_in" in cfg.taps else None

    actT = P.sbuf("actT", [128, 16 * T], BF16)
    NF_AR = 14336
    NB_AR = 38912
    ar_f = P.sbuf("ar_f", [128, NF_AR], F32)
    ar_b = P.sbuf("ar_b", [128, NB_AR], BF16)
    AR = Arena(ar_f, ar_b, NF_AR, NB_AR)
    cb = P.sbuf("cb", [128, 4, 128], BF16)
    ident = cb[:, 0, :]
    tri = cb[:, 1, :]
    negc = cb[:, 2, :]
    negw = cb[:, 3, :]
    lconst = P.sbuf("lconst_sb", [128, NL * 8], F32)
    lbt = P.sbuf("lbt", [128, 6 * 4], F32)
    lbs = P.sbuf("lbs", [128, 8], F32)
    smallf = P.sbuf("smallf", [128, 384], F32)
    gB_sb = P.sbuf("gB_sb", [128, 768], F32)
    identf = P.sbuf("identf", [32, 32], F32)
    pf = [P.psum("pf%d" % i, [128, 512], F32) for i in range(8)]
    epsb = P.sbuf("epsb", [128, 1], F32)
    P.op("dve", lambda e: e.memset(epsb[:], EPS), writes=["epsb"])

    def pbf(i, c0=0, n=1024):
        return pf[i][:, :].bitcast(BF16)[:, c0:c0 + n]

    hT3 = actT[:, :].rearrange("p (c t) -> p c t", c=16)

    P.dma("sp", cb[:], c_b_d[:, :, :], writes=["cb"], chan="cb")
    phz = P.sbuf("phz", [128, NT, 2], F32)
    P.dma("sp", phz[:], c_phz_d[:, :, :], writes=["phz"], chan="phz")
    P.dma("sp", lconst[:].rearrange("p (l k) -> p l k", l=NL), lconst_d.rearrange("l p k -> p l k"),
          writes=["lconst"], chan="lconst")
    P.dma("sp", identf[:], c_identf_d[:, :], writes=["identf"], chan="identf")
    hg_sb = P.sbuf("hg_sb", [24, 128], F32)
    P.dma("sp", hg_sb[:], hg_gamma.rearrange("l (h p) -> (l h) p", p=128), writes=["hg_sb"], chan="hg_sb")
    P.op("pe", lambda e: e.matmul(pf[0][:, 0:24], lhsT=hg_sb[:], rhs=identf[0:24, 0:24], start=True, stop=True),
         reads=["hg_sb", "identf"], writes=["pf0"])
    P.op("dve", lambda e: e.tensor_copy(out=lbt[:], in_=pf[0][:, 0:24]), reads=["pf0"], writes=["lbt"])
    P.op("act", lambda e: e.activation(out=lbt[:], in_=lbt[:], func=AF.Exp), reads=["lbt"], writes=["lbt"])
    P.op("dve", lambda e: e.tensor_reduce(out=lbs[:, 0:6], in_=lbt[:].rearrange("p (l h) -> p h l", l=4),
                                          axis=AX.X, op=ALU.add), reads=["lbt"], writes=["lbs"])
    P.op("dve", lambda e: e.reciprocal(out=lbs[:, 0:6], in_=lbs[:, 0:6]), reads=["lbs"], writes=["lbs"])

    evac_rr = [0]

    def evac(out_ap, in_ap, reads, writes):
        evac_rr[0] += 1
        if evac_rr[0] % 2:
            return P.op("act", lambda e: e.copy(out=out_ap, in_=in_ap), reads=reads, writes=writes)
        return P.op("dve", lambda e: e.tensor_copy(out=out_ap, in_=in_ap), reads=reads, writes=writes)

    def rstd_ops(ss_ap, n, res):
        P.op("act", lambda e: e.activation(out=ss_ap, in_=ss_ap, func=AF.Ln, bias=epsb[0:ss_ap.shape[0], :], scale=1.0 / n),
             reads=[res], writes=[res])
        P.op("act", lambda e: e.activation(out=ss_ap, in_=ss_ap, func=AF.Exp, scale=-0.5), reads=[res], writes=[res])

    def norm_to_T(tag, xsrc, gvec, dstT, tok0, ntile, gbc, xt, hn, ssb):
        for i in range(ntile):
            s = i % 2
            P.dma("sp", xt[s], xsrc[i * 128:(i + 1) * 128, :], writes=[tag + "xt%d" % s], chan=tag + "xt%d" % s)
            P.op("act", lambda e, s=s, i=i: e.activation(out=hn[s], in_=xt[s], func=AF.Square,
                                                         accum_out=ssb[:, i:i + 1]),
                 reads=[tag + "xt%d" % s], writes=[tag + "hn%d" % s, tag + "ss%d" % i])
            rstd_ops(ssb[:, i:i + 1], D, tag + "ss%d" % i)
            P.op("dve", lambda e, s=s, i=i: e.scalar_tensor_tensor(out=hn[s], in0=xt[s], scalar=ssb[:, i:i + 1],
                                                                   in1=gbc, op0=ALU.mult, op1=ALU.mult),
                 reads=[tag + "xt%d" % s, tag + "ss%d" % i, tag + "gbc"], writes=[tag + "hn%d" % s])
            for g in range(4):
                bank = 6 + (g % 2)
                for k in range(4):
                    c = g * 4 + k
                    P.op("pe", lambda e, s=s, c=c, k=k, bank=bank: e.transpose(
                        out=pbf(bank, k * 128, 128), in_=hn[s][:, c * 128:(c + 1) * 128], identity=ident),
                        reads=[tag + "hn%d" % s, "cb"], writes=["pf%d" % bank])
                t0 = tok0 + i * 128
                evac(dstT[:, g * 4:(g + 1) * 4, t0:t0 + 128],
                     pbf(bank, 0, 512).rearrange("p (k t) -> p k t", k=4),
                     reads=["pf%d" % bank], writes=[tag + "dstT"])

    def phase_pre(li, xsrc):
        AR.reset()
        tag = "pre."
        xt = [AR.f32([128, D]) for _ in range(2)]
        gbc = AR.f32([128, D])
        fst = [AR.f32([128, T]) for _ in range(2)]
        hn = [AR.bf([128, D]) for _ in range(2)]
        wb = [AR.bf([128, 16, 512]) for _ in range(3)]
        bst = [AR.bf([128, T]) for _ in range(2)]
        tst_b = [AR.bf([128, 512]) for _ in range(2)]
        tst_f = [AR.f32([128, 512]) for _ in range(2)]
        tst_v = [AR.bf([128, 4, VP]) for _ in range(2)]
        for i_ in range(2):
            P.op("pool", lambda e, i_=i_: e.memset(tst_v[i_][:, :, 128:VP], 0.0), writes=[tag + "tstv%d" % i_])
            P.op("pool", lambda e, i_=i_: e.memset(tst_v[i_][:, :, 128:129], 1.0), reads=[tag + "tstv%d" % i_], writes=[tag + "tstv%d" % i_])
        tvi = 0
        ssb = smallf[:, 0:16]
        P.op("dve", lambda e: e.memset(ssb, 0.0), writes=[tag + "ss%d" % i for i in range(16)])
        P.dma("sp", gbc, W["attn_norm"][li].partition_broadcast(128), writes=[tag + "gbc"], chan=tag + "gbc")
        norm_to_T(tag, xsrc, None, hT3, 0, NT, gbc, xt, hn, ssb)
        wv = W["w_in"][li].rearrange("(c p) n -> p c n", p=128)
        pi = 0
        fmi = 0
        tmi = 0
        bank_rr = 0
        for (name, c0, ncol, kind, dt) in SEG:
            off = 0
            while off < ncol:
                nco = min(512, ncol - off)
                s = pi % 3
                pi += 1
                wres = tag + "wb%d" % s
                P.dma("pool", wb[s][:, :, 0:nco], wv[:, :, c0 + off:c0 + off + nco], writes=[wres], chan=wres)
                if kind == "FM":
                    for jt in range(nco // 128):
                        tile_id = (off // 128) + jt
                        if dt == "b":
                            st = bst[fmi % 2]
                            stres = tag + "bst%d" % (fmi % 2)
                            dst = fmb[FMB_BASE[name] + tile_id]
                        else:
                            st = fst[fmi % 2]
                            stres = tag + "fst%d" % (fmi % 2)
                            dst = fmf[FMF_BASE[name] + tile_id]
                        fmi += 1
                        for tb in range(T // 512):
                            bank = bank_rr % 4
                            bank_rr += 1
                            for c in range(16):
                                P.op("pe", lambda e, s=s, c=c, jt=jt, tb=tb, bank=bank: e.matmul(
                                    pf[bank][:, :], lhsT=wb[s][:, c, jt * 128:(jt + 1) * 128],
                                    rhs=hT3[:, c, tb * 512:(tb + 1) * 512], start=(c == 0), stop=(c == 15)),
                                    reads=[wres, tag + "dstT"], writes=["pf%d" % bank])
                            evac(st[:, tb * 512:(tb + 1) * 512], pf[bank][:, :], reads=["pf%d" % bank], writes=[stres])
                        P.dma("sp", dst, st, reads=[stres], chan=stres)
                else:
                    for it in range(NT):
                        bank = bank_rr % 4
                        bank_rr += 1
                        for c in range(16):
                            P.op("pe", lambda e, s=s, c=c, it=it, bank=bank, nco=nco: e.matmul(
                                pf[bank][:, 0:nco], lhsT=hT3[:, c, it * 128:(it + 1) * 128],
                                rhs=wb[s][:, c, 0:nco], start=(c == 0), stop=(c == 15)),
                                reads=[wres, tag + "dstT"], writes=["pf%d" % bank])
                        nj = nco // 128
                        j0 = off // 128
                        if name in TMV_BASE:
                            st = tst_v[tvi % 2]
                            stres = tag + "tstv%d" % (tvi % 2)
                            tvi += 1
                            evac(st[:, 0:nj, 0:128], pf[bank][:, 0:nco].rearrange("p (j c) -> p j c", c=128),
                                 reads=["pf%d" % bank], writes=[stres])
                            t0_ = TMV_BASE[name] + j0
                            P.dma("sp", tmv[t0_:t0_ + nj, :, it, :].rearrange("j p c -> p j c"), st[:, 0:nj, :],
                                  reads=[stres], chan=stres)
                            continue
                        if dt == "b":
                            st = tst_b[tmi % 2]
                            stres = tag + "tstb%d" % (tmi % 2)
                        else:
                            st = tst_f[tmi % 2]
                            stres = tag + "tstf%d" % (tmi % 2)
                        tmi += 1
                        evac(st[:, 0:nco], pf[bank][:, 0:nco], reads=["pf%d" % bank], writes=[stres])
                        if False:
                            pass
                        elif name in ("b_i", "b_g"):
                            dd = tmbi if name == "b_i" else tmbg
                            for half in range(2):
                                P.dma("sp", dd[j0:j0 + nj, :, 2 * it + half, :].rearrange("j p c -> p j c"),
                                      st[64 * half:64 * half + 64, 0:nco].rearrange("p (j c) -> p j c", c=128),
                                      reads=[stres], chan=stres)
                        else:
                            for g_ in range(2):
                                P.dma("sp", tmg[g_, :, it, :], st[:, 9 * g_:9 * g_ + 9], reads=[stres], chan=stres)
                off += nco
        P.barrier()

    def yT_store(chunk, t0, n, in_ap, scale_ap, in_res):
        o_ap = hT3[:, chunk, t0:t0 + n]
        if scale_ap is None:
            P.op("act", lambda e: e.copy(out=o_ap, in_=in_ap), reads=in_res, writes=["yT.%d" % chunk])
        else:
            P.op("act", lambda e: e.activation(out=o_ap, in_=in_ap, func=AF.Copy, scale=scale_ap),
                 reads=in_res, writes=["yT.%d" % chunk])

    def phase_A(li):
        AR.reset()
        tag = "A."
        qT = [AR.bf([128, T]) for _ in range(2)]
        kT = [AR.bf([128, T]) for _ in range(2)]
        va = [AR.bf([128, NT, VP]) for _ in range(2)]
        PT = [AR.bf([128, 512]) for _ in range(4)]
        onb = [AR.bf([128, 128]) for _ in range(2)]
        lamb = AR.f32([128, 4, 64])
        lamt = AR.f32([128, 2, 64])
        osb = [AR.f32([128, 128]) for _ in range(2)]
        t1 = [AR.f32([128, 128]) for _ in range(2)]
        sqj = AR.f32([128, 128])
        sm = smallf
        lam = sm[:, 0:1]
        nlam = sm[:, 1:2]
        s12 = sm[:, 2:4]
        gA = sm[:, 4:8]
        ss2 = sm[:, 16:80]
        r2 = sm[:, 80:208]
        rl = sm[:, 208:216]
        lc = lconst[:, li * 8:(li + 1) * 8]
        for k, nm in enumerate(["da_lam_q1", "da_lam_k1", "da_lam_q2", "da_lam_k2"]):
            P.dma("sp", lamb[:, k, :], W[nm][li].partition_broadcast(128), writes=[tag + "lamb%d" % k], chan=tag + "lamb%d" % k)
        P.op("dve", lambda e: e.tensor_tensor(out=lamt[:, 0, :], in0=lamb[:, 0, :], in1=lamb[:, 1, :], op=ALU.mult),
             reads=[tag + "lamb0", tag + "lamb1"], writes=[tag + "lamt"])
        P.op("dve", lambda e: e.tensor_tensor(out=lamt[:, 1, :], in0=lamb[:, 2, :], in1=lamb[:, 3, :], op=ALU.mult),
             reads=[tag + "lamb2", tag + "lamb3", tag + "lamt"], writes=[tag + "lamt"])
        P.op("dve", lambda e: e.tensor_reduce(out=s12, in_=lamt[:, :, :], axis=AX.X, op=ALU.add),
             reads=[tag + "lamt"], writes=[tag + "s12"])
        P.op("act", lambda e: e.activation(out=s12, in_=s12, func=AF.Exp), reads=[tag + "s12"], writes=[tag + "s12"])
        P.op("dve", lambda e: e.tensor_tensor(out=lam, in0=sm[:, 2:3], in1=sm[:, 3:4], op=ALU.subtract),
             reads=[tag + "s12"], writes=[tag + "lam"])
        P.op("dve", lambda e: e.tensor_scalar(out=lam, in0=lam, scalar1=lc[:, 0:1], scalar2=None, op0=ALU.add),
             reads=[tag + "lam", "lconst"], writes=[tag + "lam"])
        P.op("dve", lambda e: e.tensor_scalar(out=nlam, in0=lam, scalar1=-1.0, scalar2=None, op0=ALU.mult),
             reads=[tag + "lam"], writes=[tag + "nlam"])
        gArow = AR.f32([128, 512])
        P.dma("sp", gArow, W["da_norm"][li].rearrange("h d -> (h d)").partition_broadcast(128), writes=[tag + "gA"], chan=tag + "gA")
        P.op("dve", lambda e: e.tensor_scalar(out=gArow, in0=gArow, scalar1=lc[:, 1:2], scalar2=None, op0=ALU.mult),
             reads=[tag + "gA", "lconst"], writes=[tag + "gA"])
        P.op("dve", lambda e: e.memset(ss2, 0.0), writes=[tag + "ss2"])
        grp_ctr = [0]
        deferred = [None]
        for h in range(cfg.lim.get("A_heads", 4)):
            hb = h % 2
            qres, kres, vres = tag + "qT%d" % hb, tag + "kT%d" % hb, tag + "va%d" % hb
            P.dma("sp", qT[hb], fmb[FMB_BASE["a_q"] + h], writes=[qres], chan=qres)
            P.dma("sp", kT[hb], fmb[FMB_BASE["a_k"] + h], writes=[kres], chan=kres)
            P.dma("sp", va[hb], tmv[TMV_BASE["a_v"] + h], writes=[vres], chan=vres)
            for i in range(cfg.lim.get("A_tiles", NT)):
                ob = 2 + (i % 2)
                groups = []
                for c in range(2):
                    for g0 in range(0, i + 1, 4):
                        g_ = grp_ctr[0]
                        grp_ctr[0] += 1
                        groups.append((c, list(range(g0, min(g0 + 4, i + 1))), g_ % 2, g_ % 4))

                def emit_S(gr, i=i, hb=hb, qres=qres, kres=kres):
                    c, kts, sb, ps = gr
                    for jj, kt in enumerate(kts):
                        P.op("pe", lambda e, c=c, kt=kt, jj=jj, sb=sb: e.matmul(
                            pf[sb][:, jj * 128:(jj + 1) * 128],
                            lhsT=kT[hb][64 * c:64 * c + 64, kt * 128:(kt + 1) * 128],
                            rhs=qT[hb][64 * c:64 * c + 64, i * 128:(i + 1) * 128], start=True, stop=True),
                            reads=[qres, kres], writes=["pf%d" % sb])

                def emit_rest(gr, i=i, hb=hb, ob=ob, vres=vres):
                    c, kts, sb, ps = gr
                    oc = pf[ob][:, c * 256:c * 256 + 129]
                    n = len(kts) * 128
                    P.op("act", lambda e: e.activation(out=PT[ps][:, 0:n], in_=pf[sb][:, 0:n], func=AF.Exp, scale=0.125),
                         reads=["pf%d" % sb], writes=[tag + "PT%d" % ps])
                    if kts[-1] == i:
                        jj = len(kts) - 1
                        P.op("pool", lambda e: e.tensor_tensor(
                            out=PT[ps][:, jj * 128:(jj + 1) * 128], in0=PT[ps][:, jj * 128:(jj + 1) * 128],
                            in1=tri, op=ALU.mult), reads=[tag + "PT%d" % ps], writes=[tag + "PT%d" % ps])
                    for jj, kt in enumerate(kts):
                        P.op("pe", lambda e, jj=jj, kt=kt: e.matmul(
                            oc, lhsT=PT[ps][:, jj * 128:(jj + 1) * 128], rhs=va[hb][:, kt, 0:129],
                            start=(kt == 0), stop=(kt == i)),
                            reads=[tag + "PT%d" % ps, vres], writes=["pf%d" % ob])

                emit_S(groups[0])
                for gi, gr in enumerate(groups):
                    if gi + 1 < len(groups):
                        emit_S(groups[gi + 1])
                    emit_rest(gr)
                    if gi == 0 and deferred[0] is not None:
                        deferred[0]()
                        deferred[0] = None
                u = h * NT + i
                ores = "pf%d" % ob
                s = i % 2
                rr = r2[:, 2 * u:2 * u + 2]
                P.op("dve", lambda e, ob=ob, rr=rr: e.reciprocal(out=rr, in_=pf[ob][:, 128:512:256]),
                     reads=[ores], writes=[tag + "r%d" % u])
                P.op("dve", lambda e, rr=rr, s=s: e.tensor_tensor(out=rl[:, s:s + 1], in0=rr[:, 1:2], in1=nlam, op=ALU.mult),
                     reads=[tag + "r%d" % u, tag + "nlam"], writes=[tag + "rl%d" % s])
                P.op("dve", lambda e, ob=ob, s=s: e.tensor_scalar(out=t1[s], in0=pf[ob][:, 256:384], scalar1=rl[:, s:s + 1],
                                                                  scalar2=None, op0=ALU.mult),
                     reads=[ores, tag + "rl%d" % s], writes=[tag + "t1%d" % s])
                P.op("dve", lambda e, ob=ob, s=s, rr=rr: e.scalar_tensor_tensor(
                    out=osb[s], in0=pf[ob][:, 0:128], scalar=rr[:, 0:1], in1=t1[s], op0=ALU.mult, op1=ALU.add),
                    reads=[ores, tag + "r%d" % u, tag + "t1%d" % s], writes=[tag + "osb%d" % s])
                P.op("act", lambda e, s=s, u=u: e.activation(out=sqj, in_=osb[s], func=AF.Square, accum_out=ss2[:, u:u + 1]),
                     reads=[tag + "osb%d" % s, tag + "ss2"], writes=[tag + "sqj", tag + "ssu%d" % u])
                rstd_ops(ss2[:, u:u + 1], 128, tag + "ssu%d" % u)
                P.op("dve", lambda e, s=s, u=u, h=h: e.scalar_tensor_tensor(out=onb[s], in0=osb[s], scalar=ss2[:, u:u + 1],
                                                                       in1=gArow[:, h * 128:(h + 1) * 128], op0=ALU.mult, op1=ALU.mult),
                     reads=[tag + "osb%d" % s, tag + "ssu%d" % u, tag + "gA"], writes=[tag + "onb%d" % s])

                def tail(s=s, i=i, h=h):
                    tb = 6 + (i % 2)
                    P.op("pe", lambda e: e.transpose(out=pbf(tb, 0, 128), in_=onb[s], identity=ident),
                         reads=[tag + "onb%d" % s], writes=["pf%d" % tb])
                    yT_store(h, i * 128, 128, pbf(tb, 0, 128), None, ["pf%d" % tb])
                deferred[0] = tail
        if deferred[0] is not None:
            deferred[0]()
            deferred[0] = None
        P.barrier()

    def phase_B(li):
        tag = "B."
        lc = lconst[:, li * 8:(li + 1) * 8]
        C = 64
        NCH = T // C
        sm = smallf
        gBrow = gB_sb[0:64, :]
        lbv = sm[:, 6:12]
        oml = sm[:, 12:13]

        def sc(k, j):
            b0 = 16 + k * 96 + j * 32
            return sm[:, b0:b0 + 32]

        for hbatch in range(cfg.lim.get("B_batches", 3)):
            AR.reset()
            heads = [hbatch * 2 + k for k in range(2)]
            NH = len(heads)
            zt = AR.f32([128, T])
            lf = AR.f32([128, T])
            cu = AR.f32([128, T])
            msk = AR.f32([128, T])
            gtmp = AR.f32([64, NCH * 128])
            Sst = [AR.f32([128, 128]) for _ in range(NH)]
            tmp2 = [[AR.f32([128, 128]) for _ in range(2)] for _ in range(NH)]
            ogf = [AR.f32([64, 128]) for _ in range(NH)]
            sqj = AR.f32([64, 128])
            ssb = [AR.f32([64, NCH]) for _ in range(NH)]
            ky = AR.bf([128, T])
            qe = [AR.bf([128, T]) for _ in range(NH)]
            ke = [AR.bf([128, T]) for _ in range(NH)]
            vv = [AR.bf([64, NCH, 128]) for _ in range(NH)]
            sg = [AR.bf([64, NCH, 128]) for _ in range(NH)]
            kd = [[AR.bf([64, 128]) for _ in range(2)] for _ in range(NH)]
            ATb = [[AR.bf([64, 64]) for _ in range(2)] for _ in range(NH)]
            Sm = [[AR.bf([128, 128]) for _ in range(2)] for _ in range(NH)]
            ybf = [[AR.bf([64, 128]) for _ in range(2)] for _ in range(NH)]
            if hbatch == 0:
                P.dma("sp", gBrow, W["hg_norm"][li].rearrange("h d -> (h d)").partition_broadcast(64), writes=[tag + "gB"], chan=tag + "gB")
                lb3 = lbt[:].rearrange("p (l h) -> p l h", l=4)
                P.op("dve", lambda e: e.tensor_scalar(out=lbv, in0=lb3[:, 0, :], scalar1=lc[:, 4:5], scalar2=None, op0=ALU.mult),
                     writes=[tag + "lbv"])
                for m in range(1, 4):
                    P.op("dve", lambda e, m=m: e.scalar_tensor_tensor(out=lbv, in0=lb3[:, m, :], scalar=lc[:, 4 + m:5 + m],
                                                                      in1=lbv, op0=ALU.mult, op1=ALU.add),
                         reads=[tag + "lbv"], writes=[tag + "lbv"])
                P.op("dve", lambda e: e.tensor_tensor(out=lbv, in0=lbv, in1=lbs[:, 0:6], op=ALU.mult),
                     reads=[tag + "lbv"], writes=[tag + "lbv"])
            P.op("pool", lambda e: e.memset(msk, 1.0), writes=[tag + "msk"])
            P.op("pool", lambda e: e.memset(msk.rearrange("p (n c) -> p n c", c=C)[:, :, 0:1], 0.0),
                 reads=[tag + "msk"], writes=[tag + "msk"])
            for k, h in enumerate(heads):
                hk = tag + "h%d." % k
                P.dma("sp", zt, fmf[FMF_BASE["b_f"] + h], writes=[tag + "zt"], chan=tag + "zt")
                P.dma("sp", vv[k], tmbi[h], writes=[hk + "vv"], chan=hk + "vv")
                P.dma("sp", gtmp.rearrange("p (n c) -> p n c", c=128), tmbg[h], writes=[tag + "gtmp"], chan=tag + "gtmp")
                P.op("act", lambda e, k=k: e.activation(out=sg[k][:, :, :].rearrange("p n c -> p (n c)"), in_=gtmp, func=AF.Silu),
                     reads=[tag + "gtmp"], writes=[hk + "sg"])
                P.op("act", lambda e: e.activation(out=zt, in_=zt, func=AF.Sigmoid), reads=[tag + "zt"], writes=[tag + "zt"])
                P.op("dve", lambda e, h=h: e.tensor_scalar(out=oml, in0=lbv[:, h:h + 1], scalar1=-1.0, scalar2=1.0,
                                                           op0=ALU.mult, op1=ALU.add),
                     reads=[tag + "lbv"], writes=[tag + "oml"])
                P.op("dve", lambda e, h=h: e.tensor_scalar(out=zt, in0=zt, scalar1=oml, scalar2=lbv[:, h:h + 1],
                                                           op0=ALU.mult, op1=ALU.add),
                     reads=[tag + "zt", tag + "oml", tag + "lbv"], writes=[tag + "zt"])
                P.op("act", lambda e: e.activation(out=lf, in_=zt, func=AF.Ln), reads=[tag + "zt"], writes=[tag + "lf"])
                P.op("pool", lambda e: e.tensor_scalar(out=ky, in0=zt, scalar1=-1.0, scalar2=1.0, op0=ALU.mult, op1=ALU.add),
                     reads=[tag + "zt"], writes=[tag + "ky"])
                P.op("dve", lambda e: e.tensor_tensor_scan(out=cu, data0=msk, data1=lf, initial=0.0, op0=ALU.mult, op1=ALU.add),
                     reads=[tag + "msk", tag + "lf"], writes=[tag + "cu"])
                cu3 = cu.rearrange("p (n c) -> p n c", c=C)
                emid, elast, elm = sc(k, 0), sc(k, 1), sc(k, 2)
                P.op("act", lambda e, emid=emid: e.activation(out=emid, in_=cu3[:, :, C // 2 - 1], func=AF.Exp),
                     reads=[tag + "cu"], writes=[hk + "emid"])
                P.op("act", lambda e, elast=elast: e.activation(out=elast, in_=cu3[:, :, C - 1], func=AF.Exp),
                     reads=[tag + "cu"], writes=[hk + "elast"])
                P.op("dve", lambda e, elm=elm: e.tensor_tensor(out=elm, in0=cu3[:, :, C - 1], in1=cu3[:, :, C // 2 - 1], op=ALU.subtract),
                     reads=[tag + "cu"], writes=[hk + "elm"])
                P.op("act", lambda e, elm=elm: e.activation(out=elm, in_=elm, func=AF.Exp), reads=[hk + "elm"], writes=[hk + "elm"])
                lf3 = lf.rearrange("p (n c) -> p n c", c=C)
                P.op("dve", lambda e: e.tensor_tensor(out=lf3, in0=cu3, in1=cu3[:, :, C // 2 - 1:C // 2].to_broadcast([128, NCH, C]),
                                                      op=ALU.subtract),
                     reads=[tag + "cu", tag + "lf"], writes=[tag + "lf"])
                P.dma("sp", zt, fmf[FMF_BASE["b_q"] + h], writes=[tag + "zt"], chan=tag + "zt")
                P.op("act", lambda e: e.activation(out=cu, in_=lf, func=AF.Exp), reads=[tag + "lf"], writes=[tag + "cu"])
                P.op("act", lambda e: e.activation(out=lf, in_=lf, func=AF.Exp, scale=-1.0), reads=[tag + "lf"], writes=[tag + "lf"])
                P.op("dve", lambda e, k=k: e.tensor_tensor(out=qe[k], in0=zt, in1=cu, op=ALU.mult),
                     reads=[tag + "zt", tag + "cu"], writes=[hk + "qe"])
                P.op("pool", lambda e, k=k: e.tensor_tensor(out=ke[k], in0=ky, in1=lf, op=ALU.mult),
                     reads=[tag + "ky", tag + "lf"], writes=[hk + "ke"])
                P.op("dve", lambda e, k=k: e.memset(Sst[k], 0.0), writes=[hk + "S"])
                P.op("dve", lambda e, k=k: e.memset(ssb[k], 0.0), writes=[hk + "ss%d" % n for n in range(NCH)])
            NCHK = cfg.lim.get("B_chunks", NCH)

            def stage1(n, k):
                hk = tag + "h%d." % k
                par = n % 2
                cs = slice(n * C, (n + 1) * C)
                tb = 6 + ((n * NH + k) % 2)
                P.op("pe", lambda e: e.transpose(out=pbf(tb, 0, 128)[0:64, :], in_=ke[k][:, cs], identity=ident),
                     reads=[hk + "ke"], writes=["pf%d" % tb])
                evac(kd[k][par], pbf(tb, 0, 128)[0:64, :], reads=["pf%d" % tb], writes=[hk + "kd%d" % par])
                P.op("pe", lambda e: e.matmul(pf[k][0:64, 0:64], lhsT=ke[k][:, cs], rhs=qe[k][:, cs], start=True, stop=True),
                     reads=[hk + "ke", hk + "qe"], writes=["pf%d" % k])
                P.op("dve", lambda e: e.tensor_tensor(out=ATb[k][par], in0=pf[k][0:64, 0:64], in1=tri[0:64, 0:64], op=ALU.mult),
                     reads=["pf%d" % k], writes=[hk + "AT%d" % par])

            def stage2(n, k, h):
                hk = tag + "h%d." % k
                par = n % 2
                cs = slice(n * C, (n + 1) * C)
                emid, elast, elm = sc(k, 0), sc(k, 1), sc(k, 2)
                bS = 2 + k
                bO = 4 + k
                oc = pf[bO][0:64, 0:128]
                ores = "pf%d" % bO
                if n > 0:
                    P.op("act", lambda e: e.activation(out=Sm[k][par], in_=Sst[k], func=AF.Copy, scale=emid[:, n:n + 1]),
                         reads=[hk + "S", hk + "emid"], writes=[hk + "Sm%d" % par])
                P.op("pe", lambda e: e.matmul(oc, lhsT=ATb[k][par], rhs=vv[k][:, n, :], start=True, stop=(n == 0)),
                     reads=[hk + "AT%d" % par, hk + "vv"], writes=[ores])
                if n > 0:
                    P.op("pe", lambda e: e.matmul(oc, lhsT=qe[k][:, cs], rhs=Sm[k][par], start=False, stop=True),
                         reads=[hk + "qe", hk + "Sm%d" % par], writes=[ores])
                if n < NCH - 1:
                    P.op("pe", lambda e: e.matmul(pf[bS][:, 0:128], lhsT=kd[k][par], rhs=vv[k][:, n, :], start=True, stop=True),
                         reads=[hk + "kd%d" % par, hk + "vv"], writes=["pf%d" % bS])
                    P.op("dve", lambda e: e.tensor_scalar(out=tmp2[k][par], in0=pf[bS][:, 0:128], scalar1=elm[:, n:n + 1],
                                                          scalar2=None, op0=ALU.mult),
                         reads=["pf%d" % bS, hk + "elm"], writes=[hk + "tmp2%d" % par])
                    P.op("dve", lambda e: e.scalar_tensor_tensor(out=Sst[k], in0=Sst[k], scalar=elast[:, n:n + 1], in1=tmp2[k][par],
                                                                 op0=ALU.mult, op1=ALU.add),
                         reads=[hk + "S", hk + "elast", hk + "tmp2%d" % par], writes=[hk + "S"])
                P.op("act", lambda e: e.activation(out=sqj, in_=oc, func=AF.Square, accum_out=ssb[k][:, n:n + 1]),
                     reads=[ores], writes=[tag + "sqj", hk + "ss%d" % n])
                rstd_ops(ssb[k][:, n:n + 1], 128, hk + "ss%d" % n)
                P.op("dve", lambda e: e.scalar_tensor_tensor(out=ogf[k], in0=oc, scalar=ssb[k][:, n:n + 1],
                                                             in1=gBrow[:, h * 128:(h + 1) * 128], op0=ALU.mult, op1=ALU.mult),
                     reads=[ores, hk + "ss%d" % n, tag + "gB"], writes=[hk + "og"])
                P.op("pool", lambda e: e.tensor_tensor(out=ybf[k][par], in0=ogf[k], in1=sg[k][:, n, :], op=ALU.mult),
                     reads=[hk + "og", hk + "sg"], writes=[hk + "yb%d" % par])

            def tailB(n, k, h):
                hk = tag + "h%d." % k
                par = n % 2
                tb2 = 6 + ((n * NH + k + 1) % 2)
                P.op("pe", lambda e: e.transpose(out=pbf(tb2, 256, 64), in_=ybf[k][par], identity=ident[0:64, 0:64]),
                     reads=[hk + "yb%d" % par], writes=["pf%d" % tb2])
                yT_store(4 + h, n * C, C, pbf(tb2, 256, 64), None, ["pf%d" % tb2])

            for k, h in enumerate(heads):
                stage1(0, k)
            for n in range(NCHK):
                if n + 1 < NCHK:
                    for k, h in enumerate(heads):
                        stage1(n + 1, k)
                for k, h in enumerate(heads):
                    stage2(n, k, h)
                if n > 0:
                    for k, h in enumerate(heads):
                        tailB(n - 1, k, h)
            for k, h in enumerate(heads):
                tailB(NCHK - 1, k, h)
            P.barrier()

    def phase_C(li):
        tag = "C."
        sm = smallf
        scale = 128.0 ** -0.5
        CT = cfg.lim.get("C_tiles", NT)
        for g in range(cfg.lim.get("C_groups", 2)):
            AR.reset()
            qT = [AR.bf([128, T]) for _ in range(3)]
            kcT = AR.bf([128, T])
            vcT = AR.bf([128, T])
            ksT = AR.bf([128, T])
            kwT = AR.bf([128, T])
            vsa = AR.bf([128, NT, VP])
            vwa = AR.bf([128, NT, VP])
            cmask = AR.bf([128, T])
            expand = AR.bf([32, T])
            kcc = AR.bf([128, 128])
            vca = AR.bf([128, 168])
            mark_b = AR.ob
            xs = AR.bf([128, 32, 127])
            w1b = AR.bf([128, 32, 256])
            w2b = AR.bf([128, 2, 128])
            glb = AR.bf([128, 256])
            glT = AR.bf([128, 2, 128])
            ocg = [AR.f32([128, NT, 128]) for _ in range(3)]
            imp = AR.f32([128, NT, 32])
            selb = AR.f32([128, NT, 32])
            glg = AR.f32([128, NT, 9])
            peT = AR.bf([128, 32])
            pe_b = AR.bf([32, 128])
            gx = [AR.f32([128, 256]) for _ in range(3)]
            sc1 = AR.f32([128, 32])
            sc2 = AR.f32([128, 32])
            acc = [AR.f32([128, 128]) for _ in range(2)]
            m8 = sm[:, 0:8]
            m8b = sm[:, 8:16]
            thr = sm[:, 16:32]
            rc = sm[:, 32:80]
            rsw = sm[:, 80:96]
            csw = sm[:, 96:112]
            rcg = sm[:, 112:160]
            for j in range(3):
                P.dma("sp", qT[j], fmb[FMB_BASE["c_q"] + g * 3 + j], reads=["scr.c_q"], writes=[tag + "qT%d" % j], chan=tag + "qT%d" % j)
            for nm, buf in (("c_kc", kcT), ("c_vc", vcT), ("c_ks", ksT), ("c_kw", kwT)):
                P.dma("sp", buf, fmb[FMB_BASE[nm] + g], reads=["scr." + nm], writes=[tag + nm], chan=tag + nm)
            for nm, buf in (("c_vs", vsa), ("c_vw", vwa)):
                P.dma("sp", buf, tmv[TMV_BASE[nm] + g], writes=[tag + nm], chan=tag + nm)
            P.dma("sp", glg, tmg[g], writes=[tag + "glg"], chan=tag + "glg")
            P.op("act", lambda e: e.activation(out=glg, in_=glg, func=AF.Sigmoid), reads=[tag + "glg"], writes=[tag + "glg"])
            P.dma("sp", cmask, c_cmask_d[:, :], writes=[tag + "cmask"], chan=tag + "cmask")
            P.dma("sp", expand, c_expand_d[:, :], writes=[tag + "expand"], chan=tag + "expand")
            P.dma("sp", selb, c_selbias_d[:, :, :], writes=[tag + "selb"], chan=tag + "selb")
            P.op("pool", lambda e: e.memset(vca, 0.0), writes=[tag + "vca"])
            P.op("pool", lambda e: e.memset(vca[:, 128:129], 1.0), reads=[tag + "vca"], writes=[tag + "vca"])
            P.dma("sp", vca[:, 129:161], c_ov_d[:, :], reads=[tag + "vca"], writes=[tag + "vca_ov"], chan=tag + "vca_ov")
            for which, srcT, pen, w1n, w2n in (("k", kcT, "nsa_pe_k", "nsa_ck_w1", "nsa_ck_w2"),
                                               ("v", vcT, "nsa_pe_v", "nsa_cv_w1", "nsa_cv_w2")):
                P.dma("pool", pe_b, W[pen][li], writes=[tag + "pe_b"], chan=tag + "pe_b")
                P.op("pe", lambda e: e.transpose(out=pbf(7, 0, 32), in_=pe_b, identity=ident[0:32, 0:32]), reads=[tag + "pe_b"], writes=["pf7"])
                evac(peT, pbf(7, 0, 32), reads=["pf7"], writes=[tag + "peT"])
                P.dma("pool", w1b, W[w1n][li].rearrange("(l d) m -> d l m", d=128), writes=[tag + "w1b"], chan=tag + "w1b")
                P.dma("pool", w2b, W[w2n][li].rearrange("(c p) d -> p c d", p=128), writes=[tag + "w2b"], chan=tag + "w2b")
                sv = srcT.rearrange("p (n r) -> p n r", r=16)
                srcres = tag + ("c_kc" if which == "k" else "c_vc")
                P.op("dve", lambda e, sv=sv: e.tensor_tensor(
                    out=xs[:, 0:16, :], in0=sv[:, 0:127, :].rearrange("p n l -> p l n"),
                    in1=peT[:, 0:16].unsqueeze(2).to_broadcast([128, 16, 127]), op=ALU.add),
                    reads=[srcres, tag + "peT"], writes=[tag + "xs"])
                P.op("dve", lambda e, sv=sv: e.tensor_tensor(
                    out=xs[:, 16:32, :], in0=sv[:, 1:128, :].rearrange("p n l -> p l n"),
                    in1=peT[:, 16:32].unsqueeze(2).to_broadcast([128, 16, 127]), op=ALU.add),
                    reads=[srcres, tag + "peT", tag + "xs"], writes=[tag + "xs"])
                for l in range(32):
                    P.op("pe", lambda e, l=l: e.matmul(pf[5][0:127, 0:256], lhsT=xs[:, l, :], rhs=w1b[:, l, :],
                                                       start=(l == 0), stop=(l == 31)),
                         reads=[tag + "xs", tag + "w1b"], writes=["pf5"])
                xg = gx[2][0:127, :]
                P.op("act", lambda e: e.copy(out=xg, in_=pf[5][0:127, 0:256]), reads=["pf5"], writes=[tag + "gx2"])
                P.op("dve", lambda e: e.tensor_tensor(out=gx[0][0:127, :], in0=xg, in1=xg, op=ALU.mult),
                     reads=[tag + "gx2"], writes=[tag + "gx0"])
                P.op("dve", lambda e: e.tensor_scalar(out=gx[0][0:127, :], in0=gx[0][0:127, :], scalar1=0.044715, scalar2=1.0,
                                                      op0=ALU.mult, op1=ALU.add), reads=[tag + "gx0"], writes=[tag + "gx0"])
                P.op("dve", lambda e: e.tensor_tensor(out=gx[0][0:127, :], in0=gx[0][0:127, :], in1=xg, op=ALU.mult),
                     reads=[tag + "gx0", tag + "gx2"], writes=[tag + "gx0"])
                P.op("act", lambda e: e.activation(out=gx[1][0:127, :], in_=gx[0][0:127, :], func=AF.Sigmoid, scale=1.5957691216057308),
                     reads=[tag + "gx0"], writes=[tag + "gx1"])
                P.op("dve", lambda e: e.tensor_tensor(out=glb[0:127, :], in0=gx[1][0:127, :], in1=xg, op=ALU.mult),
                     reads=[tag + "gx1", tag + "gx2"], writes=[tag + "glb"])
                for c in range(2):
                    P.op("pe", lambda e, c=c: e.transpose(out=pbf(6, c * 128, 127), in_=glb[0:127, c * 128:(c + 1) * 128],
                                                          identity=ident[0:127, 0:127]),
                         reads=[tag + "glb", "cb"], writes=["pf6"])
                evac(glT[:, :, 0:127], pbf(6, 0, 256).rearrange("p (c n) -> p c n", c=2)[:, :, 0:127], reads=["pf6"], writes=[tag + "glT"])
                if which == "k":
                    for c in range(2):
                        P.op("pe", lambda e, c=c: e.matmul(pf[5][:, 256:383], lhsT=w2b[:, c, :], rhs=glT[:, c, 0:127],
                                                           start=(c == 0), stop=(c == 1)),
                             reads=[tag + "w2b", tag + "glT"], writes=["pf5"])
                    evac(kcc[:, 0:127], pf[5][:, 256:383], reads=["pf5"], writes=[tag + "kcc"])
                else:
                    for c in range(2):
                        P.op("pe", lambda e, c=c: e.matmul(pf[5][0:127, 256:384], lhsT=glT[:, c, 0:127], rhs=w2b[:, c, :],
                                                           start=(c == 0), stop=(c == 1)),
                             reads=[tag + "w2b", tag + "glT"], writes=["pf5"])
                    evac(vca[0:127, 0:128], pf[5][0:127, 256:384], reads=["pf5", tag + "vca"], writes=[tag + "vca_v"])
            P.barrier()
            AR.ob = mark_b
            PTc = [AR.bf([128, 512]) for _ in range(2)]
            PT = [AR.bf([128, 512]) for _ in range(4)]
            nsel = AR.bf([128, 32])
            nselT = AR.bf([32, NT, 128])
            accb = [AR.bf([128, 128]) for _ in range(2)]
            cnt = 0
            NQB = (CT + 3) // 4
            cjobs = [(j, qb) for j in range(3) for qb in range(NQB)]

            def emit_Sc(idx):
                j, qb = cjobs[idx]
                sb = idx % 2
                P.op("pe", lambda e: e.matmul(pf[sb][0:127, :], lhsT=kcc[:, 0:127], rhs=qT[j][:, qb * 512:(qb + 1) * 512],
                                              start=True, stop=True),
                     reads=[tag + "kcc", tag + "qT%d" % j], writes=["pf%d" % sb])

            emit_Sc(0)
            for j in range(3):
                for qb in range(NQB):
                    sb = cnt % 2
                    pc = cnt % 2
                    cnt += 1
                    if cnt < len(cjobs):
                        emit_Sc(cnt)
                    P.op("act", lambda e, sb=sb, pc=pc: e.activation(out=PTc[pc][0:127, :], in_=pf[sb][0:127, :], func=AF.Exp, scale=scale),
                         reads=["pf%d" % sb], writes=[tag + "PTc%d" % pc])
                    P.op("pool", lambda e, pc=pc, qb=qb: e.tensor_tensor(out=PTc[pc][0:127, :], in0=PTc[pc][0:127, :],
                                                                         in1=cmask[0:127, qb * 512:(qb + 1) * 512], op=ALU.mult),
                         reads=[tag + "PTc%d" % pc, tag + "cmask"], writes=[tag + "PTc%d" % pc])
                    for it4 in range(4):
                        it = qb * 4 + it4
                        half = it % 2
                        oc = pf[4 + half][:, 0:161]
                        ores = "pf%d" % (4 + half)
                        P.op("pe", lambda e, pc=pc, it4=it4, oc=oc: e.matmul(oc, lhsT=PTc[pc][0:127, it4 * 128:(it4 + 1) * 128],
                                                                             rhs=vca[0:127, 0:161], start=True, stop=True),
                             reads=[tag + "PTc%d" % pc, tag + "vca", tag + "vca_ov", tag + "vca_v"], writes=[ores])
                        u = j * NT + it
                        P.op("dve", lambda e, oc=oc, u=u: e.tensor_scalar(out=rc[:, u:u + 1], in0=oc[:, 128:129], scalar1=1e-30, scalar2=None,
                                                                          op0=ALU.max), reads=[ores], writes=[tag + "rc%d" % u])
                        P.op("dve", lambda e, u=u: e.reciprocal(out=rc[:, u:u + 1], in_=rc[:, u:u + 1]),
                             reads=[tag + "rc%d" % u], writes=[tag + "rc%d" % u])
                        if j == 0:
                            P.op("dve", lambda e, oc=oc, u=u, it=it: e.tensor_scalar(out=imp[:, it, :], in0=oc[:, 129:161], scalar1=rc[:, u:u + 1],
                                                                                     scalar2=None, op0=ALU.mult),
                                 reads=[ores, tag + "rc%d" % u], writes=[tag + "imp%d" % it])
                        else:
                            P.op("dve", lambda e, oc=oc, u=u, it=it: e.scalar_tensor_tensor(out=imp[:, it, :], in0=oc[:, 129:161], scalar=rc[:, u:u + 1],
                                                                                            in1=imp[:, it, :], op0=ALU.mult, op1=ALU.add),
                                 reads=[ores, tag + "rc%d" % u, tag + "imp%d" % it], writes=[tag + "imp%d" % it])
                        P.op("dve", lambda e, u=u, it=it, j=j: e.tensor_tensor(out=rcg[:, u:u + 1], in0=rc[:, u:u + 1], in1=glg[:, it, 3 * j:3 * j + 1], op=ALU.mult),
                             reads=[tag + "rc%d" % u, tag + "glg"], writes=[tag + "rcg%d" % u])
                        if "c" not in cfg.lim.get("C_terms", "csw"):
                            P.op("dve", lambda e, u=u: e.memset(rcg[:, u:u + 1], 0.0), reads=[tag + "rcg%d" % u], writes=[tag + "rcg%d" % u])
                        P.op("act", lambda e, oc=oc, u=u, it=it, j=j: e.activation(out=ocg[j][:, it, :], in_=oc[:, 0:128], func=AF.Copy, scale=rcg[:, u:u + 1]),
                             reads=[ores, tag + "rcg%d" % u], writes=[tag + "ocg%d.%d" % (j, it)])
            for it in range(CT):
                P.op("dve", lambda e, it=it: e.tensor_tensor(out=sc1, in0=imp[:, it, :], in1=selb[:, it, :], op=ALU.add),
                     reads=[tag + "imp%d" % it, tag + "selb"], writes=[tag + "sc1"])
                P.op("dve", lambda e: e.max(out=m8, in_=sc1), reads=[tag + "sc1"], writes=[tag + "m8"])
                P.op("dve", lambda e: e.match_replace(out=sc2, in_to_replace=m8, in_values=sc1, imm_value=-1.0e30),
                     reads=[tag + "sc1", tag + "m8"], writes=[tag + "sc2"])
                P.op("dve", lambda e: e.max(out=m8b, in_=sc2), reads=[tag + "sc2"], writes=[tag + "m8b"])
                P.op("dve", lambda e, it=it: e.tensor_reduce(out=thr[:, it:it + 1], in_=m8b, axis=AX.X, op=ALU.min),
                     reads=[tag + "m8b"], writes=[tag + "thr%d" % it])
                P.op("dve", lambda e, it=it: e.tensor_scalar(out=nsel, in0=sc1, scalar1=thr[:, it:it + 1], scalar2=None, op0=ALU.is_lt),
                     reads=[tag + "sc1", tag + "thr%d" % it], writes=[tag + "nsel"])
                P.op("pe", lambda e: e.transpose(out=pbf(6, 512, 128)[0:32, :], in_=nsel, identity=ident),
                     reads=[tag + "nsel", "cb"], writes=["pf6"])
                evac(nselT[:, it, :], pbf(6, 512, 128)[0:32, :], reads=["pf6"], writes=[tag + "nselT%d" % it])
            grp_ctr = [0]
            deferred = [None]
            for j in range(3):
                for i in range(CT):
                    ob = 2 + (i % 2)
                    o_s = pf[ob][:, 0:129]
                    o_w = pf[ob][:, 256:385]
                    ores = "pf%d" % ob
                    groups = []
                    for branch in ("s", "w"):
                        kts_all = list(range(0, i + 1)) if branch == "s" else list(range(max(0, i - 4), i + 1))
                        for g0 in range(0, len(kts_all), 4):
                            g_ = grp_ctr[0]
                            grp_ctr[0] += 1
                            groups.append((branch, kts_all[g0:g0 + 4], kts_all[0], g_ % 2, g_ % 4))

                    def emit_S(gr, i=i, j=j):
                        branch, kts, k0, sb, ps = gr
                        kTb = ksT if branch == "s" else kwT
                        for jj, kt in enumerate(kts):
                            extra = []
                            if branch == "s":
                                extra.append("sel")
                            if kt == i:
                                extra.append("negc")
                            if branch == "w" and kt == i - 4:
                                extra.append("negw")
                            dst = pf[sb][:, jj * 128:(jj + 1) * 128]
                            P.op("pe", lambda e, kt=kt, dst=dst, ne=len(extra): e.matmul(
                                dst, lhsT=kTb[:, kt * 128:(kt + 1) * 128], rhs=qT[j][:, i * 128:(i + 1) * 128],
                                start=True, stop=(ne == 0)), writes=["pf%d" % sb])
                            for xi, kind in enumerate(extra):
                                last = (xi == len(extra) - 1)
                                if kind == "sel":
                                    P.op("pe", lambda e, kt=kt, dst=dst, last=last: e.matmul(
                                        dst, lhsT=expand[0:32, kt * 128:(kt + 1) * 128], rhs=nselT[0:32, i, :], start=False, stop=last),
                                        reads=[tag + "nselT%d" % i], writes=["pf%d" % sb])
                                else:
                                    mk = negc if kind == "negc" else negw
                                    P.op("pe", lambda e, dst=dst, last=last, mk=mk: e.matmul(dst, lhsT=ident, rhs=mk, start=False, stop=last),
                                         writes=["pf%d" % sb])

                    def emit_rest(gr, i=i, o_s=o_s, o_w=o_w, ores=ores):
                        branch, kts, k0, sb, ps = gr
                        vab = vsa if branch == "s" else vwa
                        oc = o_s if branch == "s" else o_w
                        n = len(kts) * 128
                        P.op("act", lambda e: e.activation(out=PT[ps][:, 0:n], in_=pf[sb][:, 0:n], func=AF.Exp, scale=scale),
                             reads=["pf%d" % sb], writes=[tag + "PT%d" % ps])
                        for jj, kt in enumerate(kts):
                            P.op("pe", lambda e, jj=jj, kt=kt: e.matmul(
                                oc, lhsT=PT[ps][:, jj * 128:(jj + 1) * 128], rhs=vab[:, kt, 0:129], start=(kt == k0), stop=(kt == i)),
                                reads=[tag + "PT%d" % ps], writes=[ores])

                    emit_S(groups[0])
                    for gi, gr in enumerate(groups):
                        if gi + 1 < len(groups):
                            emit_S(groups[gi + 1])
                        emit_rest(gr)
                        if gi == 0 and deferred[0] is not None:
                            deferred[0]()
                            deferred[0] = None
                    s = (j * NT + i) % 2
                    u = j * NT + i
                    rs = rsw[:, 2 * s:2 * s + 2]
                    cs_ = csw[:, 2 * s:2 * s + 2]
                    P.op("dve", lambda e, ob=ob, rs=rs, i=i: e.tensor_tensor(out=rs, in0=pf[ob][:, 128:512:256], in1=phz[:, i, :], op=ALU.add),
                         reads=[ores], writes=[tag + "rs%d" % s])
                    P.op("dve", lambda e, rs=rs: e.reciprocal(out=rs, in_=rs),
                         reads=[tag + "rs%d" % s], writes=[tag + "rs%d" % s])
                    P.op("dve", lambda e, rs=rs, cs_=cs_, i=i, j=j: e.tensor_tensor(out=cs_, in0=rs, in1=glg[:, i, 3 * j + 1:3 * j + 3], op=ALU.mult),
                         reads=[tag + "rs%d" % s], writes=[tag + "cs%d" % s])
                    if "s" not in cfg.lim.get("C_terms", "csw"):
                        P.op("dve", lambda e, cs_=cs_: e.memset(cs_[:, 0:1], 0.0), reads=[tag + "cs%d" % s], writes=[tag + "cs%d" % s])
                    if "w" not in cfg.lim.get("C_terms", "csw"):
                        P.op("dve", lambda e, cs_=cs_: e.memset(cs_[:, 1:2], 0.0), reads=[tag + "cs%d" % s], writes=[tag + "cs%d" % s])
                    P.op("dve", lambda e, s=s, cs_=cs_, j=j, i=i, o_s=o_s: e.scalar_tensor_tensor(
                        out=acc[s], in0=o_s[:, 0:128], scalar=cs_[:, 0:1], in1=ocg[j][:, i, :], op0=ALU.mult, op1=ALU.add),
                        reads=[ores, tag + "cs%d" % s], writes=[tag + "acc%d" % s])
                    P.op("dve", lambda e, s=s, cs_=cs_, o_w=o_w: e.scalar_tensor_tensor(
                        out=accb[s], in0=o_w[:, 0:128], scalar=cs_[:, 1:2], in1=acc[s], op0=ALU.mult, op1=ALU.add),
                        reads=[ores, tag + "cs%d" % s, tag + "acc%d" % s], writes=[tag + "accb%d" % s])

                    def tail(s=s, u=u, i=i, j=j):
                        tb = 6 + (u % 2)
                        P.op("pe", lambda e: e.transpose(out=pbf(tb, 0, 128), in_=accb[s], identity=ident),
                             reads=[tag + "accb%d" % s], writes=["pf%d" % tb])
                        yT_store(10 + g * 3 + j, i * 128, 128, pbf(tb, 0, 128), None, ["pf%d" % tb])
                    deferred[0] = tail
            if deferred[0] is not None:
                deferred[0]()
                deferred[0] = None
            P.barrier()

    def phase_post(li, xsrc, xdst):
        tag = "post."
        AR.reset()
        wb = [AR.bf([128, 16, 512]) for _ in range(2)]
        xs_ = [AR.f32([128, 512]) for _ in range(3)]
        wv = W["w_out"][li].rearrange("(c p) n -> p c n", p=128)
        k = 0
        for pn in range(4):
            s = pn % 2
            wres = tag + "wo%d" % s
            P.dma("pool", wb[s], wv[:, :, pn * 512:(pn + 1) * 512], writes=[wres], chan=wres)
            for it in range(NT):
                bank = k % 4
                xs = k % 3
                k += 1
                xres = tag + "xs%d" % xs
                P.dma("sp", xs_[xs], xsrc[it * 128:(it + 1) * 128, pn * 512:(pn + 1) * 512], writes=[xres], chan=xres)
                for c in range(16):
                    P.op("pe", lambda e, s=s, c=c, it=it, bank=bank: e.matmul(
                        pf[bank][:, :], lhsT=hT3[:, c, it * 128:(it + 1) * 128], rhs=wb[s][:, c, :], start=(c == 0), stop=(c == 15)),
                        reads=[wres] + ["yT.%d" % cc for cc in ([c] if True else [])], writes=["pf%d" % bank])
                P.op("dve", lambda e, xs=xs, bank=bank: e.tensor_tensor(out=xs_[xs], in0=xs_[xs], in1=pf[bank][:, :], op=ALU.add),
                     reads=["pf%d" % bank, xres], writes=[xres])
                P.dma("sp", xmid[it * 128:(it + 1) * 128, pn * 512:(pn + 1) * 512], xs_[xs], reads=[xres], chan=xres)
        P.barrier()
        AR.reset()
        TB = 512
        NTB = TB // 128
        hT2 = actT[:, 0:16 * TB].rearrange("p (c t) -> p c t", c=16)
        uT = actT[:, 16 * TB:16 * TB + 44 * TB].rearrange("p (c t) -> p c t", c=44)
        xt = [AR.f32([128, D]) for _ in range(2)]
        gbc = AR.f32([128, D])
        xo = [AR.f32([128, 512]) for _ in range(3)]
        hn = [AR.bf([128, D]) for _ in range(2)]
        wg = [AR.bf([128, 16, 256]) for _ in range(2)]
        wu = [AR.bf([128, 16, 256]) for _ in range(2)]
        wd = [AR.bf([128, 11, 512]) for _ in range(3)]
        sgt = [AR.bf([128, 512]) for _ in range(2)]
        ssb = smallf[:, 0:16]
        P.dma("sp", gbc, W["ffn_norm"][li].partition_broadcast(128), writes=[tag + "gbc"], chan=tag + "gbc")
        wgv = W["w_gate"][li].rearrange("(c p) n -> p c n", p=128)
        wuv = W["w_up"][li].rearrange("(c p) n -> p c n", p=128)
        wdv = W["w_down"][li].rearrange("(c p) n -> p c n", p=128)
        kk = 0
        kd_ = 0
        for blk in range(T // TB):
            t0 = blk * TB
            P.op("dve", lambda e: e.memset(ssb, 0.0), writes=[tag + "ss%d" % i for i in range(NTB)])
            norm_to_T(tag, xmid[t0:t0 + TB, :], None, hT2, 0, NTB, gbc, xt, hn, ssb)
            for pn in range(DFF // 256):
                s = pn % 2
                gres, ures = tag + "wg%d" % s, tag + "wu%d" % s
                P.dma("pool", wg[s], wgv[:, :, pn * 256:(pn + 1) * 256], writes=[gres], chan=gres)
                P.dma("pool", wu[s], wuv[:, :, pn * 256:(pn + 1) * 256], writes=[ures], chan=ures)
                for jt in range(2):
                    ft = pn * 2 + jt
                    bg = (kk % 2) * 2
                    bu = bg + 1
                    sgi = kk % 2
                    kk += 1
                    for c in range(16):
                        P.op("pe", lambda e, s=s, c=c, jt=jt, bg=bg: e.matmul(pf[bg][:, :], lhsT=wg[s][:, c, jt * 128:(jt + 1) * 128],
                                                                              rhs=hT2[:, c, :], start=(c == 0), stop=(c == 15)),
                             reads=[gres, tag + "dstT"], writes=["pf%d" % bg])
                    for c in range(16):
                        P.op("pe", lambda e, s=s, c=c, jt=jt, bu=bu: e.matmul(pf[bu][:, :], lhsT=wu[s][:, c, jt * 128:(jt + 1) * 128],
                                                                              rhs=hT2[:, c, :], start=(c == 0), stop=(c == 15)),
                             reads=[ures, tag + "dstT"], writes=["pf%d" % bu])
                    P.op("act", lambda e, bg=bg, sgi=sgi: e.activation(out=sgt[sgi], in_=pf[bg][:, :], func=AF.Silu),
                         reads=["pf%d" % bg], writes=[tag + "sg%d" % sgi])
                    P.op("dve", lambda e, bu=bu, sgi=sgi, ft=ft: e.tensor_tensor(out=uT[:, ft, :], in0=sgt[sgi], in1=pf[bu][:, :], op=ALU.mult),
                         reads=["pf%d" % bu, tag + "sg%d" % sgi], writes=[tag + "uT"])
            for pn in range(4):
                for half in range(4):
                    s = kd_ % 3
                    kd_ += 1
                    dres = tag + "wd%d" % s
                    P.dma("pool", wd[s], wdv[:, half * 11:(half + 1) * 11, pn * 512:(pn + 1) * 512], writes=[dres], chan=dres)
                    for it in range(NTB):
                        bank = 4 + it
                        for c in range(11):
                            cc = half * 11 + c
                            P.op("pe", lambda e, s=s, c=c, cc=cc, it=it, bank=bank: e.matmul(
                                pf[bank][:, :], lhsT=uT[:, cc, it * 128:(it + 1) * 128], rhs=wd[s][:, c, :],
                                start=(cc == 0), stop=(cc == 43)),
                                reads=[dres, tag + "uT"], writes=["pf%d" % bank])
                for it in range(NTB):
                    bank = 4 + it
                    xi = (pn * NTB + it) % 3
                    xres = tag + "xo%d" % xi
                    r0 = t0 + it * 128
                    P.dma("sp", xo[xi], xmid[r0:r0 + 128, pn * 512:(pn + 1) * 512], writes=[xres], chan=xres)
                    P.op("dve", lambda e, xi=xi, bank=bank: e.tensor_tensor(out=xo[xi], in0=xo[xi], in1=pf[bank][:, :], op=ALU.add),
                         reads=["pf%d" % bank, xres], writes=[xres])
                    P.dma("sp", xdst[r0:r0 + 128, pn * 512:(pn + 1) * 512], xo[xi], reads=[xres], chan=xres)
        P.barrier()

    def phase_final(xsrc):
        tag = "fin."
        AR.reset()
        xt = [AR.f32([128, D]) for _ in range(2)]
        gbc = AR.f32([128, D])
        yo = [AR.f32([128, D]) for _ in range(2)]
        sq = AR.bf([128, D])
        ssb = smallf[:, 0:16]
        P.op("dve", lambda e: e.memset(ssb, 0.0), writes=[tag + "ss%d" % i for i in range(16)])
        P.dma("sp", gbc, final_norm.partition_broadcast(128), writes=[tag + "gbc"], chan=tag + "gbc")
        for i in range(NT):
            s = i % 2
            P.dma("sp", xt[s], xsrc[i * 128:(i + 1) * 128, :], writes=[tag + "xt%d" % s], chan=tag + "xt%d" % s)
            P.op("act", lambda e, s=s, i=i: e.activation(out=sq, in_=xt[s], func=AF.Square, accum_out=ssb[:, i:i + 1]),
                 reads=[tag + "xt%d" % s], writes=[tag + "sq", tag + "ss%d" % i])
            rstd_ops(ssb[:, i:i + 1], D, tag + "ss%d" % i)
            P.op("dve", lambda e, s=s, i=i: e.scalar_tensor_tensor(out=yo[s], in0=xt[s], scalar=ssb[:, i:i + 1], in1=gbc,
                                                                   op0=ALU.mult, op1=ALU.mult),
                 reads=[tag + "xt%d" % s, tag + "ss%d" % i, tag + "gbc"], writes=[tag + "yo%d" % s])
            P.dma("sp", out[i * 128:(i + 1) * 128, :], yo[s], reads=[tag + "yo%d" % s], writes=["out.%d" % i], chan=tag + "yo%d" % s)
        P.op("sp", None, reads=["out.%d" % i for i in range(NT)])

    if cfg.lim or yT_dbg is not None:
        P.op("pool", lambda e: e.memset(actT[:, :], 0.0), writes=["actT0"])
    P.barrier()
    xcur = x_in
    for li in range(NL):
        last = (li == NL - 1)
        if "pre" in cfg.phases:
            phase_pre(li, xcur)
        if "yT_in" in cfg.taps:
            P.dma("sp", hT3, yT_in[:, :, :], writes=["yTall"], chan="yTin")
            P.barrier()
        if "A" in cfg.phases:
            phase_A(li)
        if "B" in cfg.phases:
            phase_B(li)
        if "C" in cfg.phases:
            phase_C(li)
        if yT_dbg is not None:
            P.dma("sp", yT_dbg[:, :, :], hT3, writes=["yTdbg"], chan="yTdbg")
            P.barrier()
        if "post" in cfg.phases:
            xdst = out if (last and not cfg.final) else xbuf[li % 2]
            phase_post(li, xcur, xdst)
            xcur = xdst
    if cfg.final:
        phase_final(xcur)
    P.barrier()
    P.emit()
    return nc, P


_CACHE = {}


def _get_prog(key, cfg):
    if key not in _CACHE:
        _CACHE[key] = build(cfg)[0]
    return _CACHE[key]


FUSED = True


def kernel(**inputs):
    inputs = {k: np.asarray(v) for k, v in inputs.items()}
    B = inputs["x"].shape[0]
    hc = host_consts()
    x = np.ascontiguousarray(inputs["x"], dtype=np.float32)
    if FUSED:
        nc = _get_prog(("fused",), Cfg(nl=DEPTH, final=True))
        lc = layer_consts(list(range(DEPTH)))
        in_maps = []
        for b in range(B):
            m = {"x": x[b], "hg_gamma": inputs["hg_gamma"], "final_norm": inputs["final_norm"], "lconst": lc}
            for n, _ in WSHAPES:
                m[n] = np.ascontiguousarray(inputs[n], dtype=np.float32)
            m.update(hc)
            in_maps.append(m)
        res = run_bass_kernel_spmd(nc, in_maps, core_ids=list(range(B)))
        return np.stack([np.asarray(res.results[b]["out"]) for b in range(B)], axis=0).astype(np.float32)
    cur = [x[b] for b in range(B)]
    for l in range(DEPTH):
        final = (l == DEPTH - 1)
        nc = _get_prog(("layer", final), Cfg(nl=1, final=final))
        lc = layer_consts([l])
        in_maps = []
        for b in range(B):
            m = {"x": np.ascontiguousarray(cur[b]), "hg_gamma": inputs["hg_gamma"], "final_norm": inputs["final_norm"],
                 "lconst": lc}
            for n, _ in WSHAPES:
                m[n] = np.ascontiguousarray(inputs[n][l:l + 1])
            m.update(hc)
            in_maps.append(m)
        res = run_bass_kernel_spmd(nc, in_maps, core_ids=list(range(B)))
        cur = [np.asarray(res.results[b]["out"]) for b in range(B)]
    return np.stack(cur, axis=0).astype(np.float32)
```
